# Optimizing a Trainium2 kernel written in Bass

```python
import math
import jax, jax.numpy as jnp
from jax import lax
import numpy as np

D_MODEL = 2048
BATCH = 2
SEQ = 16384
DEPTH = 4

GRID_W = 64
CTX_LEN = 256
ATT_HEADS = 8
ATT_QK_DIM = 64
ATT_V_DIM = 2 * ATT_QK_DIM
ATT_WIDTH = ATT_HEADS * ATT_V_DIM
Q_BLOCK = 128
ROPE_BASE = 10000.0
SGU_CHUNK = 128
SGU_GROUPS = 8
SGU_WIDTH = 1024
CONV_WIDTH = 1024
CONV_K = 31
IN_COLS = 3 * ATT_WIDTH + 2 * SGU_WIDTH + 2 * CONV_WIDTH
N_BRANCH = 3
FFN_DIM = 5632
N_EXPERTS = 8
TOP_K = 2
EXPERT_DIM = 2816
N_DENSE = (DEPTH + 1) // 2
N_MOE = DEPTH // 2
EPS = 1e-6

kernel_name = "hybrid_diffattn_sgu_conformer_moe_dit"


def rmsnorm(x, g):
    xf = x.astype(jnp.float32)
    y = xf * lax.rsqrt(jnp.mean(xf * xf, axis=-1, keepdims=True) + EPS)
    return y.astype(x.dtype) * g


def layernorm(x, g, b):
    xf = x.astype(jnp.float32)
    mu = jnp.mean(xf, axis=-1, keepdims=True)
    var = jnp.mean(jnp.square(xf - mu), axis=-1, keepdims=True)
    return ((xf - mu) * lax.rsqrt(var + EPS)).astype(x.dtype) * g + b


def modulate(h, shift, scale):
    return h * (1 + scale) + shift


def axial_angles(n):
    rows = n // GRID_W
    row = jnp.broadcast_to(jnp.arange(rows, dtype=jnp.int32)[:, None], (rows, GRID_W)).reshape(-1)
    col = jnp.broadcast_to(jnp.arange(GRID_W, dtype=jnp.int32)[None, :], (rows, GRID_W)).reshape(-1)
    half = ATT_QK_DIM // 2
    inv = ROPE_BASE ** (-jnp.arange(0, half, 2, dtype=jnp.float32) / half)
    ang_r = row.astype(jnp.float32)[:, None] * inv
    ang_c = col.astype(jnp.float32)[:, None] * inv
    return (jnp.cos(ang_r), jnp.sin(ang_r), jnp.cos(ang_c), jnp.sin(ang_c))


def _rot(x, cos, sin):
    x1, x2 = jnp.split(x, 2, axis=-1)
    return jnp.concatenate([x1 * cos - x2 * sin, x2 * cos + x1 * sin], axis=-1)


def rope2d(x, rope):
    cr, sr, cc, sc = [a[None, :, None, None, :].astype(x.dtype) for a in rope]
    xr, xc = jnp.split(x, 2, axis=-1)
    return jnp.concatenate([_rot(xr, cr, sr), _rot(xc, cc, sc)], axis=-1)


def heads_qk(t):
    return t.reshape(t.shape[0], t.shape[1], ATT_HEADS, 2, ATT_QK_DIM)


def heads_v(t):
    return t.reshape(t.shape[0], t.shape[1], ATT_HEADS, ATT_V_DIM)


def split_proj(p):
    return jnp.split(p, [ATT_WIDTH, 2 * ATT_WIDTH, 3 * ATT_WIDTH, 3 * ATT_WIDTH + 2 * SGU_WIDTH], axis=-1)


def diff_probs(q, k, lam):
    s = jnp.einsum('bqhcd,bkhcd->bhcqk', q, k).astype(jnp.float32) * (ATT_QK_DIM ** -0.5)
    p = jax.nn.softmax(s, axis=-1)
    return p[:, :, 0] - lam * p[:, :, 1]


def latent_attention(q, k, v, lam):
    b, s = q.shape[0], q.shape[1]
    nblk = s // Q_BLOCK
    qb = q.reshape(b, nblk, Q_BLOCK, ATT_HEADS, 2, ATT_QK_DIM).swapaxes(0, 1)

    def one_block(qi):
        a = diff_probs(qi, k, lam).astype(v.dtype)
        return jnp.einsum('bhqk,bkhd->bqhd', a, v)

    o = lax.map(one_block, qb)
    return o.swapaxes(0, 1).reshape(b, s, ATT_HEADS, ATT_V_DIM)


def diff_post(o, subln_g, lam_init):
    o = rmsnorm(o, subln_g) * (1 - lam_init)
    return o.reshape(o.shape[0], o.shape[1], ATT_WIDTH)


def spatial_gating(gm, ln_g, ln_b, w_s, b_s):
    b, n = gm.shape[0], gm.shape[1]
    u, vv = jnp.split(jax.nn.gelu(gm), 2, axis=-1)
    vv = layernorm(vv, ln_g, ln_b)
    vv = vv.reshape(b, n // SGU_CHUNK, SGU_CHUNK, SGU_GROUPS, SGU_WIDTH // SGU_GROUPS)
    mixed = jnp.einsum('gpq,bnqgc->bnpgc', w_s, vv) + b_s.T[None, None, :, :, None]
    return u * mixed.reshape(b, n, SGU_WIDTH)


def conformer_conv(cv, w_dw, b_dw, ln_g, ln_b):
    a, g = jnp.split(cv, 2, axis=-1)
    h = a * jax.nn.sigmoid(g)
    h = lax.conv_general_dilated(h, w_dw[:, None, :], window_strides=(1,),
                                 padding=[(CONV_K // 2, CONV_K // 2)],
                                 dimension_numbers=('NWC', 'WIO', 'NWC'),
                                 feature_group_count=CONV_WIDTH) + b_dw
    return jax.nn.silu(layernorm(h, ln_g, ln_b))


def merge_branches(h, att, sgu, conv, w_att_out, w_sgu_out, w_conv_out, w_gate, b_gate, w_o):
    g = jax.nn.sigmoid((h @ w_gate + b_gate).astype(jnp.float32)).astype(h.dtype)
    g_att, g_sgu, g_conv = jnp.split(g, N_BRANCH, axis=-1)
    y = g_att * (att @ w_att_out) + g_sgu * (sgu @ w_sgu_out) + g_conv * (conv @ w_conv_out)
    return y @ w_o


def swiglu(h, w1, w3, w2):
    return (jax.nn.silu(h @ w1) * (h @ w3)) @ w2


def moe_swiglu(h, w_r, b_r, w1, w3, w2):
    logits = (h @ w_r + b_r).astype(jnp.float32)
    top_v, top_i = lax.top_k(logits, TOP_K)
    wts = jax.nn.softmax(top_v, axis=-1)
    gate = jnp.sum(jax.nn.one_hot(top_i, N_EXPERTS, dtype=jnp.float32) * wts[..., None], axis=-2).astype(h.dtype)
    y = jnp.zeros_like(h)
    for e in range(N_EXPERTS):
        y = y + gate[..., e:e + 1] * swiglu(h, w1[e], w3[e], w2[e])
    return y


def setup_inputs(seed: int = 0) -> dict:
    key = jax.random.key(seed)
    ks = iter(jax.random.split(key, 64))

    def nrm(shape, scale):
        return jax.random.normal(next(ks), shape, dtype=jnp.float32) * scale

    def gain(shape):
        return 1.0 + nrm(shape, 0.02)

    D = D_MODEL
    return {
        "x": nrm((BATCH, SEQ, D), 1.0),
        "c": nrm((BATCH, D), 1.0),
        "ctx": nrm((BATCH, CTX_LEN, D), 1.0),
        "c_ctx": nrm((D,), 1.0),
        "w_mod": nrm((DEPTH, D, 6 * D), 0.01),
        "b_mod": nrm((DEPTH, 6 * D), 0.02),
        "norm1_g": gain((DEPTH, D)),
        "norm2_g": gain((DEPTH, D)),
        "w_in": nrm((DEPTH, D, IN_COLS), D ** -0.5),
        "lam_q1": nrm((DEPTH, ATT_QK_DIM), 0.1),
        "lam_k1": nrm((DEPTH, ATT_QK_DIM), 0.1),
        "lam_q2": nrm((DEPTH, ATT_QK_DIM), 0.1),
        "lam_k2": nrm((DEPTH, ATT_QK_DIM), 0.1),
        "subln_g": gain((DEPTH, ATT_V_DIM)),
        "w_att_out": nrm((DEPTH, ATT_WIDTH, D), ATT_WIDTH ** -0.5),
        "sgu_ln_g": gain((DEPTH, SGU_WIDTH)),
        "sgu_ln_b": nrm((DEPTH, SGU_WIDTH), 0.02),
        "w_spatial": nrm((DEPTH, SGU_GROUPS, SGU_CHUNK, SGU_CHUNK), SGU_CHUNK ** -0.5),
        "b_spatial": gain((DEPTH, SGU_GROUPS, SGU_CHUNK)),
        "w_sgu_out": nrm((DEPTH, SGU_WIDTH, D), SGU_WIDTH ** -0.5),
        "conv_w": nrm((DEPTH, CONV_K, CONV_WIDTH), CONV_K ** -0.5),
        "conv_b": nrm((DEPTH, CONV_WIDTH), 0.02),
        "conv_ln_g": gain((DEPTH, CONV_WIDTH)),
        "conv_ln_b": nrm((DEPTH, CONV_WIDTH), 0.02),
        "w_conv_out": nrm((DEPTH, CONV_WIDTH, D), CONV_WIDTH ** -0.5),
        "w_gate": nrm((DEPTH, D, N_BRANCH * D), D ** -0.5),
        "b_gate": nrm((DEPTH, N_BRANCH * D), 0.02),
        "w_o": nrm((DEPTH, D, D), D ** -0.5),
        "ffn_w1": nrm((N_DENSE, D, FFN_DIM), D ** -0.5),
        "ffn_w3": nrm((N_DENSE, D, FFN_DIM), D ** -0.5),
        "ffn_w2": nrm((N_DENSE, FFN_DIM, D), FFN_DIM ** -0.5),
        "router_w": nrm((N_MOE, D, N_EXPERTS), D ** -0.5),
        "router_b": nrm((N_MOE, N_EXPERTS), 0.01),
        "moe_w1": nrm((N_MOE, N_EXPERTS, D, EXPERT_DIM), D ** -0.5),
        "moe_w3": nrm((N_MOE, N_EXPERTS, D, EXPERT_DIM), D ** -0.5),
        "moe_w2": nrm((N_MOE, N_EXPERTS, EXPERT_DIM, D), EXPERT_DIM ** -0.5),
        "final_g": gain((D,)),
    }


def reference(x, c, ctx, c_ctx, w_mod, b_mod, norm1_g, norm2_g, w_in, lam_q1, lam_k1, lam_q2, lam_k2,
              subln_g, w_att_out, sgu_ln_g, sgu_ln_b, w_spatial, b_spatial, w_sgu_out, conv_w, conv_b,
              conv_ln_g, conv_ln_b, w_conv_out, w_gate, b_gate, w_o, ffn_w1, ffn_w3, ffn_w2,
              router_w, router_b, moe_w1, moe_w3, moe_w2, final_g):
    rope = axial_angles(x.shape[1])
    xc = ctx
    for i in range(DEPTH):
        last = i == DEPTH - 1
        m_lat = (jax.nn.silu(c) @ w_mod[i] + b_mod[i])[:, None, :]
        m_ctx = (jax.nn.silu(c_ctx) @ w_mod[i] + b_mod[i])[None, None, :]
        sh1, sc1, g1, sh2, sc2, g2 = jnp.split(m_lat, 6, axis=-1)
        csh1, csc1, cg1, csh2, csc2, cg2 = jnp.split(m_ctx, 6, axis=-1)

        lam_init = 0.8 - 0.6 * math.exp(-0.3 * i)
        lam = (jnp.exp(jnp.sum(lam_q1[i] * lam_k1[i]).astype(jnp.float32))
               - jnp.exp(jnp.sum(lam_q2[i] * lam_k2[i]).astype(jnp.float32)) + lam_init)

        def mix(h, att, gm, cv):
            return merge_branches(
                h, diff_post(att, subln_g[i], lam_init),
                spatial_gating(gm, sgu_ln_g[i], sgu_ln_b[i], w_spatial[i], b_spatial[i]),
                conformer_conv(cv, conv_w[i], conv_b[i], conv_ln_g[i], conv_ln_b[i]),
                w_att_out[i], w_sgu_out[i], w_conv_out[i], w_gate[i], b_gate[i], w_o[i])

        def ffn(h):
            if i % 2 == 0:
                j = i // 2
                return swiglu(h, ffn_w1[j], ffn_w3[j], ffn_w2[j])
            j = i // 2
            return moe_swiglu(h, router_w[j], router_b[j], moe_w1[j], moe_w3[j], moe_w2[j])

        h_lat = modulate(rmsnorm(x, norm1_g[i]), sh1, sc1)
        h_ctx = modulate(rmsnorm(xc, norm1_g[i]), csh1, csc1)
        if last:
            kc, vc = jnp.split(h_ctx @ w_in[i][:, ATT_WIDTH:3 * ATT_WIDTH], 2, axis=-1)
        else:
            qc, kc, vc, gmc, cvc = split_proj(h_ctx @ w_in[i])
        kc, vc = heads_qk(kc), heads_v(vc)

        ql, kl, vl, gml, cvl = split_proj(h_lat @ w_in[i])
        ql = rope2d(heads_qk(ql), rope)
        kl = rope2d(heads_qk(kl), rope)
        k_all = jnp.concatenate([kl, kc], axis=1)
        v_all = jnp.concatenate([heads_v(vl), vc], axis=1)
        att_l = latent_attention(ql, k_all, v_all, lam)
        y_lat = mix(h_lat, att_l, gml, cvl)
        x = x + g1 * y_lat

        if not last:
            att_c = jnp.einsum('bhqk,bkhd->bqhd', diff_probs(heads_qk(qc), kc, lam).astype(vc.dtype), vc)
            xc = xc + cg1 * mix(h_ctx, att_c, gmc, cvc)

        x = x + g2 * ffn(modulate(rmsnorm(x, norm2_g[i]), sh2, sc2))
        if not last:
            xc = xc + cg2 * ffn(modulate(rmsnorm(xc, norm2_g[i]), csh2, csc2))

    return rmsnorm(x, final_g)
```

```python
import contextlib
import math
import numpy as np
import ml_dtypes
import concourse.bass as bass
import concourse.mybir as mybir
from concourse.bass_utils import run_bass_kernel_spmd

F32 = mybir.dt.float32
BF16 = mybir.dt.bfloat16
AF = mybir.ActivationFunctionType
ALU = mybir.AluOpType
AX = mybir.AxisListType

D = 2048
DC = D // 128
BATCH = 2
GRID_W = 64
CTX = 256
HEADS = 8
QK = 64
ATT_W = 1024
SGU_W = 1024
CONV_W = 1024
CONV_K = 31
HALO = CONV_K // 2
IN_COLS = 7168
FFN_DIM = 5632
NEXP = 8
EDIM = 2816
EC = EDIM // 128
EPS = 1e-6
ROPE_BASE = 10000.0
NCORES = 8
GROUP = 4
RG4 = [[0, 1, 2, 3], [4, 5, 6, 7]]


DEBUG = {}


def _seg(name):
    return ("seg" not in DEBUG) or (name in DEBUG["seg"])


class Cfg:
    def __init__(self, seq=16384, depth=4):
        self.seq = seq
        self.depth = depth
        self.s_loc = seq // GROUP
        self.nt = self.s_loc + CTX
        self.n_dense = (depth + 1) // 2
        self.n_moe = depth // 2


class Ev:
    __slots__ = ("sem", "val", "owner")

    def __init__(self, sem, val, owner=None):
        self.sem, self.val, self.owner = sem, val, owner


class Buf:
    __slots__ = ("w", "r", "dsem", "gsem", "name")

    def __init__(self, name=""):
        self.w = None
        self.r = {}
        self.dsem = None
        self.gsem = None
        self.name = name


class Eng:
    def __init__(self, kb, e, name, is_pe=False):
        self.kb, self.e, self.name, self.is_pe = kb, e, name, is_pe
        self.sem = kb.new_sem("p_" + name)
        self.n = 0
        self.seen = {}

    def wait(self, ev, war=False):
        if ev is None:
            return
        if ev.owner is self and (self.is_pe or war):
            return
        k = id(ev.sem)
        if self.seen.get(k, 0) >= ev.val:
            return
        self.e.wait_ge(ev.sem, ev.val)
        self.seen[k] = ev.val

    def mark(self, ins):
        self.n += 1
        ins.then_inc(self.sem, 1)
        return Ev(self.sem, self.n, self)


class KB:
    def __init__(self, nc):
        self.nc = nc
        self.nsem = 0
        self.PE = Eng(self, nc.tensor, "pe", is_pe=True)
        self.ACT = Eng(self, nc.scalar, "act")
        self.DVE = Eng(self, nc.vector, "dve")
        self.POOL = Eng(self, nc.gpsimd, "pool")
        self.SP = Eng(self, nc.sync, "sp")
        self.dpool = []
        self.gpool = []
        self.dactive = []
        self.gactive = []

    def new_sem(self, name):
        self.nsem += 1
        return self.nc.semaphore(f"{name}_{self.nsem}").__enter__()

    def _deps(self, eng, reads, writes):
        for b in reads:
            eng.wait(b.w)
        for b in writes:
            eng.wait(b.w, war=True)
            for ev in b.r.values():
                eng.wait(ev, war=True)

    def _commit(self, ev, key, reads, writes):
        for b in reads:
            b.r[key] = ev
        for b in writes:
            b.w = ev
            b.r = {}

    def op(self, eng, fn, reads=(), writes=()):
        self._deps(eng, reads, writes)
        ins = fn()
        ev = eng.mark(ins)
        self._commit(ev, id(eng), reads, writes)
        return ev

    def dma(self, q, out, in_, reads=(), writes=(), sb=None):
        self._deps(q, reads, writes)
        if sb is None:
            sb = writes[0] if writes else reads[0]
        sw = (q is self.POOL)
        rec = sb.gsem if sw else sb.dsem
        if rec is None:
            pool = self.gpool if sw else self.dpool
            rec = pool.pop() if pool else [self.new_sem("g" if sw else "d"), 0]
            (self.gactive if sw else self.dactive).append(rec)
            if sw:
                sb.gsem = rec
            else:
                sb.dsem = rec
        ins = q.e.dma_start(out=out, in_=in_)
        rec[1] += 16
        ins.then_inc(rec[0], 16)
        ev = Ev(rec[0], rec[1], None)
        self._commit(ev, id(rec[0]), reads, writes)
        return ev
        self._commit(ev, id(sb.dsem[0]), reads, writes)
        return ev

    def barrier(self, scratch):
        P = self.POOL
        for E in (self.PE, self.ACT, self.DVE):
            if E.n:
                P.wait(Ev(E.sem, E.n, E))
        for rec in self.dactive + self.gactive:
            P.wait(Ev(rec[0], rec[1], None))
        ins = self.nc.gpsimd.memset(scratch, 0.0)
        ev = P.mark(ins)
        for E in (self.PE, self.ACT, self.DVE, self.SP):
            E.wait(ev)
        self.dpool.extend(self.dactive)
        self.dactive = []
        self.gpool.extend(self.gactive)
        self.gactive = []


def tiles_of(n0, n1, step):
    out = []
    t = n0
    while t < n1:
        out.append((t, min(step, n1 - t)))
        t += step
    return out


SM_LAYOUT = {}


def _sm_layout():
    off = 0
    lay = {}

    def add(name, n):
        nonlocal off
        lay[name] = (off, n)
        off += n

    add("bmod", 96)
    add("g1", 16)
    add("g2", 16)
    add("bgate", 48)
    add("convb", 8)
    add("clng", 8)
    add("clnb", 8)
    add("convw", 8 * CONV_K)
    add("subg", 128)
    add("slng", 1024)
    add("slnb", 1024)
    add("bsp", 8 * 128)
    add("lam", 4 * 64)
    add("rb", 8)
    lay["_w"] = off
    return lay


SM = _sm_layout()
C_ID = 0
C_ONES = 128
C_RSW = 256
C_SEL = 384
C_NEGH = 392
C_FING = 400
C_W = 416


class Prog:
    def __init__(self, cfg, stop_after=None):
        self.cfg = cfg
        self.stop_after = stop_after
        nc = bass.Bass("TRN2", target_bir_lowering=False)
        self.nc = nc
        self.kb = KB(nc)
        self.build()

    def dram_in(self, name, shape, dt=F32):
        return self.nc.dram_tensor(name, list(shape), dt, kind="ExternalInput").ap()

    def dram(self, name, shape, dt):
        return self.nc.dram_tensor(name, list(shape), dt).ap()

    def sb(self, es, name, shape, dt):
        self._uid = getattr(self, "_uid", 0) + 1
        return es.enter_context(self.nc.sbuf_tensor(f"s{self._uid}_{name}", list(shape), dt))

    def ps(self, es, name, shape, dt):
        self._uid = getattr(self, "_uid", 0) + 1
        return es.enter_context(self.nc.psum_tensor(f"p{self._uid}_{name}", list(shape), dt))

    def build(self):
        cfg, nc, kb = self.cfg, self.nc, self.kb
        NT, SL, DEPTH = cfg.nt, cfg.s_loc, cfg.depth
        self.xT_in = self.dram_in("xT_in", [D, NT])
        self.cT_in = self.dram_in("cT", [128, DC, 2])
        self.cos_in = self.dram_in("ropec", [128, NT])
        self.sin_in = self.dram_in("ropes", [128, NT])
        self.consts_in = self.dram_in("consts", [128, C_W])
        self.sm_in = self.dram_in("small", [DEPTH, 128, SM["_w"]])
        self.wsp_in = self.dram_in("wspT", [DEPTH, 128, 8, 128])
        G = GROUP
        self.w_mod = self.dram_in("w_mod", [DEPTH, D, 6 * D // G])
        self.w_in = self.dram_in("w_in", [DEPTH, D // G, IN_COLS])
        self.w_att_out = self.dram_in("w_att_out", [DEPTH, ATT_W // G, D])
        self.w_sgu_out = self.dram_in("w_sgu_out", [DEPTH, SGU_W // G, D])
        self.w_conv_out = self.dram_in("w_conv_out", [DEPTH, CONV_W // G, D])
        self.w_gate = self.dram_in("w_gate", [DEPTH, D // G, 3 * D])
        self.w_o = self.dram_in("w_o", [DEPTH, D // G, D])
        self.ffn_w1 = self.dram_in("ffn_w1", [cfg.n_dense, D // G, FFN_DIM])
        self.ffn_w3 = self.dram_in("ffn_w3", [cfg.n_dense, D // G, FFN_DIM])
        self.ffn_w2 = self.dram_in("ffn_w2", [cfg.n_dense, FFN_DIM // G, D])
        if cfg.n_moe:
            self.router_w = self.dram_in("router_wT", [cfg.n_moe, 128, DC, NEXP])
            self.moe_w1 = self.dram_in("moe_w1", [cfg.n_moe, NEXP, D // G, EDIM])
            self.moe_w3 = self.dram_in("moe_w3", [cfg.n_moe, NEXP, D // G, EDIM])
            self.moe_w2 = self.dram_in("moe_w2", [cfg.n_moe, NEXP, EDIM // G, D])
        self.out = nc.dram_tensor("outT", [D, SL], F32, kind="ExternalOutput").ap()
        self.xT = self.dram("xT", [D, NT], F32)
        self.hT = self.dram("hT", [D, NT], BF16)
        self.qT = self.dram("qT", [ATT_W, NT], BF16)
        self.kT_loc_h = [nc.dram_tensor(f"kT_loc{h}", [128, SL], BF16) for h in range(HEADS)]
        self.kT_all_h = [nc.dram_tensor(f"kT_all{h}", [GROUP * 128, SL], BF16) for h in range(HEADS)]
        self.kT_ctx = self.dram("kT_ctx", [ATT_W, CTX], BF16)
        self.VR = min(512, SL)
        self.NVP = SL // self.VR
        self.v_loc_p = [nc.dram_tensor(f"v_loc{p}", [self.VR, ATT_W], BF16) for p in range(self.NVP)]
        self.v_all_p = [nc.dram_tensor(f"v_all{p}", [GROUP * self.VR, ATT_W], BF16) for p in range(self.NVP)]
        self.v_ctx = self.dram("v_ctx", [CTX, ATT_W], BF16)
        self.edge_t = nc.dram_tensor("edge", [CONV_W, 32], BF16)
        self.edge_all_t = nc.dram_tensor("edge_all", [GROUP * CONV_W, 32], BF16)
        self.mod_src_t = nc.dram_tensor("mod_src", [128, 48], F32)
        self.mod_all_t = nc.dram_tensor("mod_all", [GROUP * 128, 48], F32)
        self.uT = self.dram("uT", [SGU_W, NT], BF16)
        self.vvn = self.dram("vvn", [NT, SGU_W], BF16)
        self.hcT = self.dram("hcT", [CONV_W, NT], BF16)
        self.attT = self.dram("attT", [ATT_W, NT], BF16)
        self.sguT = self.dram("sguT", [SGU_W, NT], BF16)
        self.convT = self.dram("convT", [CONV_W, NT], BF16)
        self.gate_d = self.dram("gate_d", [NT, NEXP], F32)
        self.wspec = {}
        self.wblk = {}
        MAXB = 1 << 20
        for i in range(DEPTH):
            j = i // 2
            spec = [(("in", i), self.w_in[i], D, IN_COLS), (("ga", i), self.w_gate[i], D, 3 * D),
                    (("ao", i), self.w_att_out[i], ATT_W, D), (("so", i), self.w_sgu_out[i], SGU_W, D),
                    (("co", i), self.w_conv_out[i], CONV_W, D), (("o", i), self.w_o[i], D, D)]
            if i % 2 == 0:
                spec += [(("f1", i), self.ffn_w1[j], D, FFN_DIM), (("f3", i), self.ffn_w3[j], D, FFN_DIM),
                         (("f2", i), self.ffn_w2[j], FFN_DIM, D)]
            else:
                for e in range(NEXP):
                    spec += [(("m1", i, e), self.moe_w1[j, e], D, EDIM), (("m3", i, e), self.moe_w3[j, e], D, EDIM),
                             (("m2", i, e), self.moe_w2[j, e], EDIM, D)]
            self.wspec[i] = []
            for (key, src, K, N) in spec:
                rows = K // GROUP
                cbw = 2048
                while rows * cbw * 2 > MAXB:
                    cbw //= 2
                blocks = []
                for c0 in range(0, N, cbw):
                    ncol = min(cbw, N - c0)
                    nm = "_".join(str(x) for x in key) + f"_{c0}"
                    srct = nc.dram_tensor("ws_" + nm, [rows, ncol], BF16)
                    dstt = nc.dram_tensor("wb_" + nm, [K, ncol], BF16)
                    self.wspec[i].append((src[:, c0:c0 + ncol], srct, dstt))
                    blocks.append((c0, ncol, dstt.ap()))
                self.wblk[key] = blocks
        self.cast_ev = [None] * DEPTH

        top = contextlib.ExitStack()
        self.top = top
        self.consts = self.sb(top, "consts", [128, C_W], F32)
        self.cbf = self.sb(top, "cbf", [128, 384], BF16)
        self.MOD = self.sb(top, "MOD", [128, DEPTH, 96, 2], F32)
        self.AM = self.sb(top, "AM", [128, DEPTH, 2, DC, 2], F32)
        self.bar_scr = self.sb(top, "barscr", [128, 2], F32)
        self.LAMT = self.sb(top, "LAMT", [128, DEPTH], F32)
        self.sT = self.sb(top, "sT", [128, DC, 2], F32)

        self.stop_after = getattr(self, "stop_after", None)
        self.issue_weights(0)
        if DEPTH > 1:
            self.issue_weights(1)
        self.run_phases()

    def issue_weights(self, i):
        nc, kb = self.nc, self.kb
        if DEBUG.get("no_gather"):
            self.cast_ev[i] = None
            return
        csem = kb.new_sem(f"cast{i}")
        cnt = 0
        for (src, srct, dstt) in self.wspec[i]:
            nc.gpsimd.dma_start(out=srct.ap()[:, :], in_=src).then_inc(csem, 16)
            cnt += 16
        nc.gpsimd.wait_ge(csem, cnt)
        gsem = kb.new_sem(f"gath{i}")
        n = 0
        for (src, srct, dstt) in self.wspec[i]:
            nc.gpsimd.collective_compute("AllGather", ALU.bypass, replica_groups=RG4,
                                         ins=[srct.ap().opt()], outs=[dstt.ap().opt()]).then_inc(gsem)
            n += 1
        self.cast_ev[i] = Ev(gsem, n, None)

    def wap(self, key, k0, krows, n0, wn):
        for (c0, ncol, ap) in self.wblk[key]:
            if c0 <= n0 < c0 + ncol:
                assert n0 + wn <= c0 + ncol, (key, n0, wn, c0, ncol)
                return ap[k0:k0 + krows, n0 - c0:n0 - c0 + wn]
        raise KeyError((key, n0))

    def run_phases(self):
        DEPTH = self.cfg.depth
        seq = [("init", self.phase_init, ())]
        for i in range(DEPTH):
            if i >= 1 and i + 1 < DEPTH:
                seq.append((f"w{i + 1}", self.issue_weights, (i + 1,)))
            seq += [(f"mod_{i}", self.phase_mod, (i,)), (f"norm1_{i}", self.phase_norm, (i, 1)), (f"inproj_{i}", self.phase_inproj, (i,)),
                    (f"exch_{i}", self.phase_exchange, (i,)), (f"attn_{i}", self.phase_attn, (i,)),
                    (f"sgu_{i}", self.phase_sgu, (i,)), (f"conv_{i}", self.phase_conv, (i,)),
                    (f"merge_{i}", self.phase_merge, (i,)), (f"norm2_{i}", self.phase_norm, (i, 2)),
                    (f"ffn_{i}", self.phase_ffn, (i,))]
        seq.append(("final", self.phase_final, ()))
        for (name, fn, args) in seq:
            fn(*args)
            if self.stop_after == name:
                self.emit_debug()
                return

    def emit_debug(self):
        nc, kb = self.nc, self.kb
        b = Buf()
        if DEBUG.get("waitw") and self.cast_ev[0] is not None:
            kb.SP.wait(self.cast_ev[0])
        for (name, ap) in self.debug_list():
            o = nc.dram_tensor("dbg_" + name, list(ap.shape), ap.dtype, kind="ExternalOutput").ap()
            rows = ap.shape[0]
            step = max(1, rows // 8)
            for r0 in range(0, rows, step):
                r1 = min(rows, r0 + step)
                kb.dma(kb.SP, o[r0:r1], ap[r0:r1], writes=[b])
        kb.barrier(self.bar_scr[:, 0:1])

    def debug_list(self):
        full = self._debug_full()
        want = DEBUG.get("dump")
        return [x for x in full if (want is None or x[0] in want)]

    def _debug_full(self):
        return [("wb_in", self.wblk["in", 0][1][2]), ("wb_o", self.wblk["o", 0][1][2]), ("xT", self.xT), ("hT", self.hT), ("qT", self.qT), ("kT_all0", self.kT_all_h[0].ap()), ("v_all0", self.v_all_p[0].ap()),
                ("kT_ctx", self.kT_ctx), ("v_ctx", self.v_ctx), ("uT", self.uT), ("vvn", self.vvn), ("hcT", self.hcT),
                ("attT", self.attT), ("sguT", self.sguT), ("convT", self.convT), ("gate", self.gate_d),
                ("edge_all", self.edge_all_t.ap())]

    def phase_init(self):
        cfg, nc, kb = self.cfg, self.nc, self.kb
        PE, ACT, DVE, POOL, SP = kb.PE, kb.ACT, kb.DVE, kb.POOL, kb.SP
        with contextlib.ExitStack() as es:
            bc = Buf("consts")
            kb.dma(SP, self.consts[:], self.consts_in[:, :], writes=[bc])
            bcb = Buf("cbf")
            kb.op(DVE, lambda: nc.vector.tensor_copy(self.cbf[:], self.consts[:, 0:384]), reads=[bc], writes=[bcb])
            bx = Buf("xcopy")
            for (r0, rs) in tiles_of(0, D, 256):
                kb.dma(SP, self.xT[r0:r0 + rs, :], self.xT_in[r0:r0 + rs, :], writes=[bx])
            cT = self.sb(es, "cT", [128, DC, 2], F32)
            bcT, bsT = Buf(), Buf()
            kb.dma(SP, cT[:], self.cT_in[:, :, :], writes=[bcT])
            kb.op(ACT, lambda: nc.scalar.activation(out=self.sT[:], in_=cT[:], func=AF.Silu), reads=[bcT], writes=[bsT])
            kb.barrier(self.bar_scr[:, 0:1])

    def phase_mod(self, i):
        cfg, nc, kb = self.cfg, self.nc, self.kb
        PE, ACT, DVE, POOL, SP = kb.PE, kb.ACT, kb.DVE, kb.POOL, kb.SP
        sT = self.sT
        NJ = 96 // GROUP
        with contextlib.ExitStack() as es:
            sm = self.sb(es, "sm0", [128, 128], F32)
            lam = self.sb(es, "lamv", [128, 256], F32)
            bsm = Buf()
            kb.dma(SP, sm[:, :], self.sm_in[i, :, 0:128], writes=[bsm])
            o, n = SM["lam"]
            kb.dma(SP, lam[:, :], self.sm_in[i, :, o:o + n], writes=[bsm])
            WN = 512
            wbufs = [self.sb(es, f"wm{k}", [128, DC, WN], F32) for k in range(2)]
            wB = [Buf(), Buf()]
            pst = self.ps(es, "psmod", [128, NJ, 2], F32)
            bps = Buf()
            bmod = Buf("MOD")
            cnt = 0
            for n0 in range(0, 6 * D // GROUP, WN):
                k = cnt % 2
                cnt += 1
                kb.dma(SP, wbufs[k][:], self.w_mod[i, :, n0:n0 + WN].rearrange("(c p) n -> p c n", p=128), writes=[wB[k]])

                def mm(k=k, n0=n0):
                    ins = None
                    for jj in range(WN // 128):
                        j = n0 // 128 + jj
                        for c in range(DC):
                            ins = nc.tensor.matmul(pst[:, j, :], wbufs[k][:, c, jj * 128:(jj + 1) * 128], sT[:, c, :],
                                                   start=(c == 0), stop=(c == DC - 1))
                    return ins
                kb.op(PE, mm, reads=[wB[k]], writes=[bps])
            raw = self.sb(es, "mraw", [128, NJ * 2], F32)
            braw = Buf()
            kb.op(DVE, lambda: nc.vector.tensor_copy(raw[:].rearrange("p (j c) -> p j c", c=2), pst[:, :, :]), reads=[bps], writes=[braw])
            kb.dma(POOL, self.mod_src_t.ap()[:, :], raw[:], reads=[braw])
            POOL.wait(braw.r[id(braw.gsem[0])])
            gsem = kb.new_sem(f"modg{i}")
            nc.gpsimd.collective_compute("AllGather", ALU.bypass, replica_groups=RG4,
                                         ins=[self.mod_src_t.ap().opt()], outs=[self.mod_all_t.ap().opt()]).then_inc(gsem)
            SP.wait(Ev(gsem, 1, None))
            allm = self.sb(es, "mall", [128, GROUP, NJ * 2], F32)
            ball = Buf()
            kb.dma(SP, allm[:], self.mod_all_t.ap().rearrange("(r p) x -> p r x", p=128), writes=[ball])
            for col in range(2):
                kb.op(DVE, lambda col=col: nc.vector.tensor_tensor(
                    out=self.MOD[:, i, :, col].rearrange("p (r j) -> p r j", r=GROUP),
                    in0=allm[:].rearrange("p r (j c) -> p r j c", c=2)[:, :, :, col],
                    in1=sm[:, 0:96].rearrange("p (r j) -> p r j", r=GROUP), op=ALU.add),
                    reads=[ball, bsm], writes=[bmod])
            for which in range(2):
                sc0 = 16 + 48 * which
                g0 = 96 + 16 * which
                for col in range(2):
                    kb.op(DVE, lambda which=which, col=col, sc0=sc0, g0=g0: nc.vector.scalar_tensor_tensor(
                        out=self.AM[:, i, which, :, col], in0=self.MOD[:, i, sc0:sc0 + 16, col], scalar=1.0,
                        in1=sm[:, g0:g0 + 16], op0=ALU.add, op1=ALU.mult), reads=[bmod, bsm], writes=[bmod])
            t1 = self.sb(es, "lt1", [128, 128], F32)
            t2 = self.sb(es, "lt2", [128, 2], F32)
            bl = Buf()
            lv = lam[:, :].rearrange("p (a b c) -> p a b c", a=2, b=2)
            kb.op(DVE, lambda: nc.vector.tensor_tensor(
                out=t1[:].rearrange("p (a b) -> p a b", a=2), in0=lv[:, :, 0, :], in1=lv[:, :, 1, :], op=ALU.mult), reads=[bsm], writes=[bl])
            kb.op(DVE, lambda: nc.vector.tensor_reduce(
                out=t2[:], in_=t1[:].rearrange("p (a b) -> p a b", a=2), axis=AX.X, op=ALU.add), reads=[bl], writes=[bl])
            kb.op(ACT, lambda: nc.scalar.activation(out=t2[:], in_=t2[:], func=AF.Exp), reads=[bl], writes=[bl])
            lam_init = 0.8 - 0.6 * math.exp(-0.3 * i)
            kb.op(DVE, lambda: nc.vector.scalar_tensor_tensor(
                out=self.LAMT[:, i:i + 1], in0=t2[:, 0:1], scalar=lam_init, in1=t2[:, 1:2], op0=ALU.add, op1=ALU.subtract),
                reads=[bl], writes=[bmod])
            kb.barrier(self.bar_scr[:, 0:1])

    def phase_norm(self, i, which):
        cfg, nc, kb = self.cfg, self.nc, self.kb
        NT, SL = cfg.nt, cfg.s_loc
        PE, ACT, DVE, POOL, SP = kb.PE, kb.ACT, kb.DVE, kb.POOL, kb.SP
        moe = (which == 2 and i % 2 == 1)
        sh0 = 0 if which == 1 else 48
        ones_bf = self.cbf[:, 128:256]
        with contextlib.ExitStack() as es:
            TS = 256
            X = [self.sb(es, f"nX{k}", [128, DC, TS], F32) for k in range(2)]
            SQ = [self.sb(es, f"nSQ{k}", [128, DC, TS], BF16) for k in range(2)]
            H32 = [self.sb(es, f"nH32{k}", [128, DC, TS], F32) for k in range(2)]
            HB = [self.sb(es, f"nHB{k}", [128, DC, TS], BF16) for k in range(2)]
            RS = [self.sb(es, f"nRS{k}", [128, TS], F32) for k in range(2)]
            TT = [self.sb(es, f"nT{k}", [128, TS], F32) for k in range(3)]
            negh = self.sb(es, "negh", [128, TS], F32)
            pss = [self.ps(es, f"nps{k}", [128, TS], F32) for k in range(2)]
            bX, bSQ, bH32, bHB, bRS, bps = ([Buf() for _ in range(2)] for _ in range(6))
            bTT = [Buf() for _ in range(3)]
            bneg = Buf()
            kb.op(POOL, lambda: nc.gpsimd.memset(negh[:], -0.5), writes=[bneg])
            if moe:
                j = i // 2
                WR = self.sb(es, "WR", [128, DC, NEXP], F32)
                RB = self.sb(es, "RB", [128, NEXP], F32)
                bWR = Buf()
                kb.dma(SP, WR[:], self.router_w[j, :, :, :], writes=[bWR])
                o, n = SM["rb"]
                kb.dma(SP, RB[:], self.sm_in[i, :, o:o + n], writes=[bWR])
                psr = self.ps(es, "psr", [128, 4, NEXP], F32)
                bpsr = Buf()
                G = [self.sb(es, f"rG{k}", [128, 4, 40], F32) for k in range(2)]
                bG = [Buf(), Buf()]
            tcount = 0
            for it, (t0, ts) in enumerate(tiles_of(0, NT, TS)):
                k = it % 2
                col = 0 if t0 < SL else 1
                kb.dma(SP, X[k][:, :, 0:ts], self.xT[:, t0:t0 + ts].rearrange("(c p) t -> p c t", p=128), writes=[bX[k]])
                for c4 in range(0, DC, 4):
                    kb.op(ACT, lambda k=k, c4=c4, ts=ts: nc.scalar.activation(
                        out=SQ[k][:, c4:c4 + 4, 0:ts], in_=X[k][:, c4:c4 + 4, 0:ts], func=AF.Square), reads=[bX[k]], writes=[bSQ[k]])

                def mm(k=k, ts=ts):
                    ins = None
                    for c in range(DC):
                        ins = nc.tensor.matmul(pss[k][:, 0:ts], ones_bf, SQ[k][:, c, 0:ts], start=(c == 0), stop=(c == DC - 1))
                    return ins
                kb.op(PE, mm, reads=[bSQ[k]], writes=[bps[k]])
                kb.op(DVE, lambda k=k, ts=ts: nc.vector.tensor_scalar(
                    out=RS[k][:, 0:ts], in0=pss[k][:, 0:ts], scalar1=1.0 / D, scalar2=EPS, op0=ALU.mult, op1=ALU.add),
                    reads=[bps[k]], writes=[bRS[k]])
                kb.op(POOL, lambda k=k, ts=ts: nc.gpsimd.tensor_tensor(
                    out=RS[k][:, 0:ts], in0=RS[k][:, 0:ts], in1=negh[:, 0:ts], op=ALU.pow), reads=[bRS[k], bneg], writes=[bRS[k]])
                for c in range(DC):
                    tk = tcount % 3
                    tcount += 1
                    kb.op(DVE, lambda k=k, c=c, tk=tk, ts=ts, col=col: nc.vector.scalar_tensor_tensor(
                        out=TT[tk][:, 0:ts], in0=X[k][:, c, 0:ts], scalar=self.AM[:, i, which - 1, c, col:col + 1],
                        in1=RS[k][:, 0:ts], op0=ALU.mult, op1=ALU.mult), reads=[bX[k], bRS[k]], writes=[bTT[tk]])
                    kb.op(ACT, lambda k=k, c=c, tk=tk, ts=ts, col=col: nc.scalar.activation(
                        out=H32[k][:, c, 0:ts], in_=TT[tk][:, 0:ts], func=AF.Identity,
                        bias=self.MOD[:, i, sh0 + c, col:col + 1], scale=1.0), reads=[bTT[tk]], writes=[bH32[k]])
                for c4 in range(0, DC, 8):
                    kb.op(POOL, lambda k=k, c4=c4, ts=ts: nc.gpsimd.tensor_copy(
                        HB[k][:, c4:c4 + 8, 0:ts], H32[k][:, c4:c4 + 8, 0:ts]), reads=[bH32[k]], writes=[bHB[k]])
                kb.dma(POOL, self.hT[:, t0:t0 + ts].rearrange("(c p) t -> p c t", p=128), HB[k][:, :, 0:ts], reads=[bHB[k]])
                if moe:
                    nb = ts // 128

                    def rmm(k=k, nb=nb):
                        ins = None
                        for b in range(nb):
                            for c in range(DC):
                                ins = nc.tensor.matmul(psr[:, b, :], H32[k][:, c, b * 128:(b + 1) * 128], WR[:, c, :],
                                                       start=(c == 0), stop=(c == DC - 1))
                        return ins
                    kb.op(PE, rmm, reads=[bH32[k], bWR], writes=[bpsr])
                    g = G[k]
                    bg = bG[k]
                    L, M1, K1, L2, M2, K2, WW = (g[:, 0:nb, 0:8], g[:, 0:nb, 8:9], g[:, 0:nb, 9:17], g[:, 0:nb, 17:25],
                                                 g[:, 0:nb, 25:26], g[:, 0:nb, 26:34], g[:, 0:nb, 34:40])
                    for b in range(nb):
                        kb.op(DVE, lambda b=b, g=g: nc.vector.tensor_tensor(out=g[:, b, 0:8], in0=psr[:, b, :], in1=RB[:], op=ALU.add),
                              reads=[bpsr, bWR], writes=[bg])
                        kb.op(DVE, lambda b=b, g=g: nc.vector.reduce_max(out=g[:, b, 8:9], in_=g[:, b, 0:8], axis=AX.X), reads=[bg], writes=[bg])
                        kb.op(DVE, lambda b=b, g=g: nc.vector.tensor_scalar(out=g[:, b, 9:17], in0=g[:, b, 0:8], scalar1=g[:, b, 8:9],
                                                                          scalar2=None, op0=ALU.is_equal), reads=[bg], writes=[bg])
                        kb.op(DVE, lambda b=b, g=g: nc.vector.scalar_tensor_tensor(out=g[:, b, 17:25], in0=g[:, b, 9:17], scalar=-1e30,
                                                                                 in1=g[:, b, 0:8], op0=ALU.mult, op1=ALU.add), reads=[bg], writes=[bg])
                        kb.op(DVE, lambda b=b, g=g: nc.vector.reduce_max(out=g[:, b, 25:26], in_=g[:, b, 17:25], axis=AX.X), reads=[bg], writes=[bg])
                        kb.op(DVE, lambda b=b, g=g: nc.vector.tensor_scalar(out=g[:, b, 26:34], in0=g[:, b, 17:25], scalar1=g[:, b, 25:26],
                                                                          scalar2=None, op0=ALU.is_equal), reads=[bg], writes=[bg])
                        kb.op(DVE, lambda b=b, g=g: nc.vector.tensor_tensor(out=g[:, b, 34:35], in0=g[:, b, 25:26], in1=g[:, b, 8:9], op=ALU.subtract),
                              reads=[bg], writes=[bg])
                        kb.op(ACT, lambda b=b, g=g: nc.scalar.activation(out=g[:, b, 35:36], in_=g[:, b, 34:35], func=AF.Exp), reads=[bg], writes=[bg])
                        kb.op(DVE, lambda b=b, g=g: nc.vector.tensor_scalar(out=g[:, b, 36:37], in0=g[:, b, 35:36], scalar1=1.0, scalar2=None, op0=ALU.add),
                              reads=[bg], writes=[bg])
                        kb.op(DVE, lambda b=b, g=g: nc.vector.reciprocal(out=g[:, b, 37:38], in_=g[:, b, 36:37]), reads=[bg], writes=[bg])
                        kb.op(DVE, lambda b=b, g=g: nc.vector.tensor_tensor(out=g[:, b, 38:39], in0=g[:, b, 35:36], in1=g[:, b, 37:38], op=ALU.mult),
                              reads=[bg], writes=[bg])
                        kb.op(DVE, lambda b=b, g=g: nc.vector.tensor_scalar(out=g[:, b, 9:17], in0=g[:, b, 9:17], scalar1=g[:, b, 37:38], scalar2=None,
                                                                          op0=ALU.mult), reads=[bg], writes=[bg])
                        kb.op(DVE, lambda b=b, g=g: nc.vector.scalar_tensor_tensor(out=g[:, b, 0:8], in0=g[:, b, 26:34], scalar=g[:, b, 38:39],
                                                                                 in1=g[:, b, 9:17], op0=ALU.mult, op1=ALU.add), reads=[bg], writes=[bg])
                    kb.dma(POOL, self.gate_d[t0:t0 + ts, :].rearrange("(b p) e -> p b e", p=128), g[:, 0:nb, 0:8], reads=[bg])
            kb.barrier(self.bar_scr[:, 0:1])

    def wload(self, dst, key, k0, kc, n0, wn, buf, layer):
        kb = self.kb
        kb.SP.wait(self.cast_ev[layer])
        kb.dma(kb.SP, dst, self.wap(key, k0, kc * 128, n0, wn).rearrange("(c p) n -> p c n", p=128), writes=[buf])

    def phase_inproj(self, i):
        cfg, nc, kb = self.cfg, self.nc, self.kb
        NT, SL = cfg.nt, cfg.s_loc
        PE, ACT, DVE, POOL, SP = kb.PE, kb.ACT, kb.DVE, kb.POOL, kb.SP
        W = ("in", i)
        rsw_bf = self.cbf[:, 256:384]
        VR = self.VR
        STS = 1536
        with contextlib.ExitStack() as es:
            HT = self.sb(es, "HT", [128, DC, STS], BF16)
            COS = self.sb(es, "COS", [128, STS], F32)
            SIN = self.sb(es, "SIN", [128, STS], F32)
            bHT, bCS = Buf(), Buf()
            WT = [self.sb(es, f"WT{k}", [128, DC, 512], BF16) for k in range(3)]
            bWT = [Buf() for _ in range(3)]
            NPS = 6
            pss = [self.ps(es, f"ips{k}", [128, 512], F32) for k in range(NPS)]
            bps = [Buf() for _ in range(NPS)]
            ps2 = [self.ps(es, f"ips2{k}", [128, 512], F32) for k in range(2)]
            bps2 = [Buf() for _ in range(2)]
            NS = 4
            S1 = [self.sb(es, f"iS1{k}", [128, 512], F32) for k in range(NS)]
            S2 = [self.sb(es, f"iS2{k}", [128, 512], F32) for k in range(NS)]
            SB = [self.sb(es, f"iSB{k}", [128, 512], BF16) for k in range(NS)]
            OB = [self.sb(es, f"iOB{k}", [128, 512], BF16) for k in range(NS)]
            bS1, bS2, bSB, bOB = ([Buf() for _ in range(NS)] for _ in range(4))
            G32 = [self.sb(es, f"iG{k}", [128, 1024], F32) for k in range(2)]
            V16 = [self.sb(es, f"iV{k}", [128, 1024], BF16) for k in range(2)]
            bG32, bV16 = [Buf(), Buf()], [Buf(), Buf()]
            ST = [self.sb(es, f"iST{k}", [128, 16], F32) for k in range(2)]
            bST = [Buf(), Buf()]
            LNG = self.sb(es, "iLNG", [128, 1024], F32)
            LNB = self.sb(es, "iLNB", [128, 1024], F32)
            negh = self.sb(es, "inegh", [128, 1], F32)
            bLN = Buf()
            o, n = SM["slng"]
            kb.dma(SP, LNG[:], self.sm_in[i, :, o:o + n], writes=[bLN])
            o, n = SM["slnb"]
            kb.dma(SP, LNB[:], self.sm_in[i, :, o:o + n], writes=[bLN])
            kb.op(POOL, lambda: nc.gpsimd.memset(negh[:], -0.5), writes=[bLN])
            cnt = {"w": 0, "ps": 0, "ps2": 0, "s": 0, "g": 0}

            def next_w():
                k = cnt["w"] % 3
                cnt["w"] += 1
                return k

            def next_ps():
                k = cnt["ps"] % NPS
                cnt["ps"] += 1
                return k

            def next_s():
                k = cnt["s"] % NS
                cnt["s"] += 1
                return k

            for (s0, ssz) in tiles_of(0, NT, STS):
                kb.dma(SP, HT[:, :, 0:ssz], self.hT[:, s0:s0 + ssz].rearrange("(c p) t -> p c t", p=128), writes=[bHT])
                kb.dma(SP, COS[:, 0:ssz], self.cos_in[:, s0:s0 + ssz], writes=[bCS])
                kb.dma(SP, SIN[:, 0:ssz], self.sin_in[:, s0:s0 + ssz], writes=[bCS])
                ttiles = []
                for (t0, ts) in tiles_of(s0, s0 + ssz, 512):
                    if t0 < SL < t0 + ts:
                        ttiles.append((t0, SL - t0))
                        ttiles.append((SL, t0 + ts - SL))
                    else:
                        ttiles.append((t0, ts))

                def gemmA(wk, wc, t0, ts):
                    pk = next_ps()

                    def mm():
                        ins = None
                        for c in range(DC):
                            ins = nc.tensor.matmul(pss[pk][:, 0:ts], WT[wk][:, c, wc * 128:(wc + 1) * 128],
                                                   HT[:, c, t0 - s0:t0 - s0 + ts], start=(c == 0), stop=(c == DC - 1))
                        return ins
                    kb.op(PE, mm, reads=[bWT[wk], bHT], writes=[bps[pk]])
                    return pk

                for n0 in (range(0, 2048, 512) if _seg('qk') else []):
                    wk = next_w()
                    self.wload(WT[wk][:], W, 0, DC, n0, 512, bWT[wk], i)
                    for wc in range(4):
                        j = n0 // 128 + wc
                        for (t0, ts) in ttiles:
                            pk = gemmA(wk, wc, t0, ts)
                            sk = next_s()
                            lo = t0 - s0
                            if DEBUG.get("norope"):
                                kb.op(ACT, lambda pk=pk, sk=sk, ts=ts: nc.scalar.activation(out=OB[sk][:, 0:ts], in_=pss[pk][:, 0:ts], func=AF.Identity),
                                      reads=[bps[pk]], writes=[bOB[sk]])
                                kb.dma(POOL, self.qT[(j % 8) * 128:(j % 8 + 1) * 128, t0:t0 + ts], OB[sk][:, 0:ts], reads=[bOB[sk]])
                                continue
                            kb.op(ACT, lambda pk=pk, sk=sk, ts=ts: nc.scalar.activation(out=SB[sk][:, 0:ts], in_=pss[pk][:, 0:ts], func=AF.Identity),
                                  reads=[bps[pk]], writes=[bSB[sk]])
                            p2 = cnt["ps2"] % 2
                            cnt["ps2"] += 1
                            kb.op(PE, lambda p2=p2, sk=sk, ts=ts: nc.tensor.matmul(ps2[p2][:, 0:ts], rsw_bf, SB[sk][:, 0:ts], start=True, stop=True),
                                  reads=[bSB[sk]], writes=[bps2[p2]])
                            kb.op(DVE, lambda pk=pk, sk=sk, ts=ts, lo=lo: nc.vector.tensor_tensor(
                                out=S1[sk][:, 0:ts], in0=pss[pk][:, 0:ts], in1=COS[:, lo:lo + ts], op=ALU.mult),
                                reads=[bps[pk], bCS, bSB[sk]], writes=[bS1[sk]])
                            kb.op(DVE, lambda p2=p2, sk=sk, ts=ts, lo=lo: nc.vector.tensor_tensor(
                                out=S2[sk][:, 0:ts], in0=ps2[p2][:, 0:ts], in1=SIN[:, lo:lo + ts], op=ALU.mult),
                                reads=[bps2[p2], bCS], writes=[bS2[sk]])
                            kb.op(DVE, lambda sk=sk, ts=ts: nc.vector.tensor_tensor(
                                out=OB[sk][:, 0:ts], in0=S1[sk][:, 0:ts], in1=S2[sk][:, 0:ts], op=ALU.add),
                                reads=[bS1[sk], bS2[sk]], writes=[bOB[sk]])
                            if j < 8:
                                dst = self.qT[j * 128:(j + 1) * 128, t0:t0 + ts]
                            elif t0 < SL:
                                dst = self.kT_loc_h[j - 8].ap()[:, t0:t0 + ts]
                            else:
                                dst = self.kT_ctx[(j - 8) * 128:(j - 7) * 128, t0 - SL:t0 - SL + ts]
                            kb.dma(POOL, dst, OB[sk][:, 0:ts], reads=[bOB[sk]])
                for half in (range(2) if _seg('v') else []):
                    wk = next_w()
                    self.wload(WT[wk][:], W, 0, DC, 2048 + half * 512, 512, bWT[wk], i)
                    for (t0, ts) in tiles_of(s0, s0 + ssz, 128):
                        pk = next_ps()

                        def mm(pk=pk, wk=wk, t0=t0):
                            ins = None
                            for c in range(DC):
                                ins = nc.tensor.matmul(pss[pk][:, :], HT[:, c, t0 - s0:t0 - s0 + 128], WT[wk][:, c, :],
                                                       start=(c == 0), stop=(c == DC - 1))
                            return ins
                        kb.op(PE, mm, reads=[bWT[wk], bHT], writes=[bps[pk]])
                        sk = next_s()
                        kb.op(ACT, lambda pk=pk, sk=sk: nc.scalar.activation(out=OB[sk][:, :], in_=pss[pk][:, :], func=AF.Identity),
                              reads=[bps[pk]], writes=[bOB[sk]])
                        if t0 < SL:
                            dst = self.v_loc_p[t0 // VR].ap()[t0 % VR:t0 % VR + 128, half * 512:(half + 1) * 512]
                        else:
                            dst = self.v_ctx[t0 - SL:t0 - SL + 128, half * 512:(half + 1) * 512]
                        kb.dma(POOL, dst, OB[sk][:, :], reads=[bOB[sk]])
                for n0 in (range(3072, 4096, 512) if _seg('u') else []):
                    wk = next_w()
                    self.wload(WT[wk][:], W, 0, DC, n0, 512, bWT[wk], i)
                    for wc in range(4):
                        j = (n0 - 3072) // 128 + wc
                        for (t0, ts) in ttiles:
                            pk = gemmA(wk, wc, t0, ts)
                            sk = next_s()
                            kb.op(ACT, lambda pk=pk, sk=sk, ts=ts: nc.scalar.activation(out=OB[sk][:, 0:ts], in_=pss[pk][:, 0:ts],
                                                                                      func=AF.Gelu_apprx_tanh), reads=[bps[pk]], writes=[bOB[sk]])
                            kb.dma(POOL, self.uT[j * 128:(j + 1) * 128, t0:t0 + ts], OB[sk][:, 0:ts], reads=[bOB[sk]])
                wk0 = next_w()
                self.wload(WT[wk0][:], W, 0, DC, 4096, 512, bWT[wk0], i)
                wk1 = next_w()
                self.wload(WT[wk1][:], W, 0, DC, 4608, 512, bWT[wk1], i)
                for (t0, ts) in (tiles_of(s0, s0 + ssz, 128) if _seg('vv') else []):
                    gk = cnt["g"] % 2
                    cnt["g"] += 1
                    pks = []
                    for half, wk in enumerate((wk0, wk1)):
                        pk = next_ps()
                        pks.append(pk)

                        def mm(pk=pk, wk=wk, t0=t0):
                            ins = None
                            for c in range(DC):
                                ins = nc.tensor.matmul(pss[pk][:, :], HT[:, c, t0 - s0:t0 - s0 + 128], WT[wk][:, c, :],
                                                       start=(c == 0), stop=(c == DC - 1))
                            return ins
                        kb.op(PE, mm, reads=[bWT[wk], bHT], writes=[bps[pk]])
                        kb.op(ACT, lambda pk=pk, gk=gk, half=half: nc.scalar.activation(
                            out=G32[gk][:, half * 512:(half + 1) * 512], in_=pss[pk][:, :], func=AF.Gelu_apprx_tanh),
                            reads=[bps[pk]], writes=[bG32[gk]])
                    st = ST[gk]
                    for half in range(2):
                        kb.op(DVE, lambda gk=gk, half=half, st=st: nc.vector.bn_stats(out=st[:, half * 6:(half + 1) * 6],
                                                                                      in_=G32[gk][:, half * 512:(half + 1) * 512]),
                              reads=[bG32[gk]], writes=[bST[gk]])
                    kb.op(DVE, lambda st=st: nc.vector.bn_aggr(out=st[:, 12:14], in_=st[:, 0:12]), reads=[bST[gk]], writes=[bST[gk]])
                    kb.op(DVE, lambda st=st: nc.vector.tensor_scalar(out=st[:, 14:15], in0=st[:, 13:14], scalar1=EPS, scalar2=None, op0=ALU.add),
                          reads=[bST[gk]], writes=[bST[gk]])
                    kb.op(POOL, lambda st=st: nc.gpsimd.tensor_tensor(out=st[:, 15:16], in0=st[:, 14:15], in1=negh[:, 0:1], op=ALU.pow),
                          reads=[bST[gk], bLN], writes=[bST[gk]])
                    kb.op(DVE, lambda gk=gk, st=st: nc.vector.tensor_scalar(out=G32[gk][:, :], in0=G32[gk][:, :], scalar1=st[:, 12:13],
                                                                          scalar2=st[:, 15:16], op0=ALU.subtract, op1=ALU.mult),
                          reads=[bST[gk], bG32[gk]], writes=[bG32[gk]])
                    kb.op(DVE, lambda gk=gk: nc.vector.tensor_tensor(out=G32[gk][:, :], in0=G32[gk][:, :], in1=LNG[:, :], op=ALU.mult),
                          reads=[bG32[gk], bLN], writes=[bG32[gk]])
                    kb.op(POOL, lambda gk=gk: nc.gpsimd.tensor_tensor(out=V16[gk][:, :], in0=G32[gk][:, :], in1=LNB[:, :], op=ALU.add),
                          reads=[bG32[gk], bLN], writes=[bV16[gk]])
                    kb.dma(POOL, self.vvn[t0:t0 + 128, :], V16[gk][:, :], reads=[bV16[gk]])
                for n0 in (range(0, 1024, 512) if _seg('cv') else []):
                    wka = next_w()
                    self.wload(WT[wka][:], W, 0, DC, 5120 + n0, 512, bWT[wka], i)
                    wkg = next_w()
                    self.wload(WT[wkg][:], W, 0, DC, 6144 + n0, 512, bWT[wkg], i)
                    for wc in range(4):
                        j = n0 // 128 + wc
                        for (t0, ts) in ttiles:
                            pa = gemmA(wka, wc, t0, ts)
                            pg = gemmA(wkg, wc, t0, ts)
                            sk = next_s()
                            kb.op(ACT, lambda pg=pg, sk=sk, ts=ts: nc.scalar.activation(out=S1[sk][:, 0:ts], in_=pss[pg][:, 0:ts], func=AF.Sigmoid),
                                  reads=[bps[pg]], writes=[bS1[sk]])
                            kb.op(DVE, lambda pa=pa, sk=sk, ts=ts: nc.vector.tensor_tensor(out=OB[sk][:, 0:ts], in0=pss[pa][:, 0:ts],
                                                                                         in1=S1[sk][:, 0:ts], op=ALU.mult),
                                  reads=[bps[pa], bS1[sk]], writes=[bOB[sk]])
                            kb.dma(POOL, self.hcT[j * 128:(j + 1) * 128, t0:t0 + ts], OB[sk][:, 0:ts], reads=[bOB[sk]])
            kb.barrier(self.bar_scr[:, 0:1])

    def phase_exchange(self, i):
        cfg, nc, kb = self.cfg, self.nc, self.kb
        SL = cfg.s_loc
        POOL = kb.POOL
        edge = self.edge_t.ap()
        be = Buf()
        kb.dma(POOL, edge[:, 0:16], self.hcT[:, 0:16], writes=[be])
        kb.dma(POOL, edge[:, 16:32], self.hcT[:, SL - 16:SL], writes=[be])
        POOL.wait(be.w)
        sem = kb.new_sem(f"cc{i}")
        pairs = [(self.kT_loc_h[h], self.kT_all_h[h]) for h in range(HEADS)]
        pairs += [(self.v_loc_p[p], self.v_all_p[p]) for p in range(self.NVP)]
        pairs += [(self.edge_t, self.edge_all_t)]
        for (src, dst) in pairs:
            nc.gpsimd.collective_compute("AllGather", ALU.bypass, replica_groups=RG4,
                                         ins=[src.ap().opt()], outs=[dst.ap().opt()]).then_inc(sem)
        nc.gpsimd.wait_ge(sem, len(pairs))
        kb.barrier(self.bar_scr[:, 0:1])

    def phase_attn(self, i):
        cfg, nc, kb = self.cfg, self.nc, self.kb
        NT, SL = cfg.nt, cfg.s_loc
        PE, ACT, DVE, POOL, SP = kb.PE, kb.ACT, kb.DVE, kb.POOL, kb.SP
        VR = self.VR
        NKL = GROUP * SL // 128
        NKC = CTX // 128
        NK = NKL + NKC
        id_bf = self.cbf[:, 0:128]
        lam_init = 0.8 - 0.6 * math.exp(-0.3 * i)
        with contextlib.ExitStack() as es:
            KT = [self.sb(es, f"aKT{k}", [128, NK * 128], BF16) for k in range(2)]
            VX = [self.sb(es, f"aVX{k}", [128, NK, 130], BF16) for k in range(2)]
            QAB = [self.sb(es, f"aQ{c}", [128, NT], BF16) for c in range(2)]
            bQ = Buf()
            bKT, bVX = ([Buf(), Buf()] for _ in range(2))
            for c in range(2):
                kb.op(POOL, lambda c=c: nc.gpsimd.memset(QAB[c][:], 0.0), writes=[bQ])
            NE = 3
            E = [[self.sb(es, f"aE{k}_{c}", [128, 512], BF16) for c in range(2)] for k in range(NE)]
            bE = [[Buf(), Buf()] for _ in range(NE)]
            pSt = [self.ps(es, f"apS{k}", [128, 2, 256], F32) for k in range(2)]
            pS = [[pSt[k][:, c, :] for c in range(2)] for k in range(2)]
            bpS = [[Buf(), Buf()] for _ in range(2)]
            pOb = [self.ps(es, f"apO{k}", [128, 512], F32) for k in range(4)]
            acc = lambda qb, c: pOb[qb * 2 + c]
            bAcc = [[Buf(), Buf()] for _ in range(4)]
            pT = self.ps(es, "apT", [128, 8, 128], BF16)
            bpT = Buf()
            SUBG = self.sb(es, "aSUBG", [128, 128], F32)
            bC = Buf()
            o, n = SM["subg"]
            kb.dma(SP, SUBG[:], self.sm_in[i, :, o:o + n], writes=[bC])
            for k in range(2):
                kb.op(POOL, lambda k=k: nc.gpsimd.memset(VX[k][:], 1.0), writes=[bVX[k]])
            SC = [self.sb(es, f"aSC{k}", [128, 16], F32) for k in range(2)]
            bSC = [Buf(), Buf()]
            OT = [self.sb(es, f"aOT{k}", [128, 128], F32) for k in range(2)]
            O2 = [self.sb(es, f"aO2{k}", [128, 128], F32) for k in range(2)]
            SQt = [self.sb(es, f"aSQ{k}", [128, 128], F32) for k in range(2)]
            AB = [self.sb(es, f"aAB{k}", [128, 128], BF16) for k in range(2)]
            bOT, bO2, bSQ, bAB = ([Buf(), Buf()] for _ in range(4))
            ATO = [self.sb(es, f"aATO{k}", [128, 512], BF16) for k in range(2)]
            bATO = [Buf(), Buf()]
            negh = self.sb(es, "anegh", [128, 1], F32)
            kb.op(POOL, lambda: nc.gpsimd.memset(negh[:], -0.5), writes=[bC])
            git = 0
            ecount = 0
            ocount = 0
            for h in range(HEADS):
                hk = h % 2
                rows = slice(h * 128, (h + 1) * 128)
                kb.dma(SP, KT[hk][:, 0:GROUP * SL].rearrange("p (r t) -> p r t", r=GROUP),
                       self.kT_all_h[h].ap().rearrange("(r p) t -> p r t", p=128), writes=[bKT[hk]])
                kb.dma(SP, KT[hk][:, GROUP * SL:GROUP * SL + CTX], self.kT_ctx[rows, :], writes=[bKT[hk]])
                tpp = VR // 128
                for p in range(self.NVP):
                    for r in range(GROUP):
                        k0 = r * (SL // 128) + p * tpp
                        kb.dma(SP, VX[hk][:, k0:k0 + tpp, 0:128],
                               self.v_all_p[p].ap()[r * VR:(r + 1) * VR, rows].rearrange("(t q) d -> q t d", q=128), writes=[bVX[hk]])
                kb.dma(SP, VX[hk][:, NKL:NK, 0:128], self.v_ctx[:, rows].rearrange("(t p) d -> p t d", p=128), writes=[bVX[hk]])
                kb.dma(SP, QAB[0][0:64, :], self.qT[h * 128:h * 128 + 64, :], writes=[bQ])
                kb.dma(SP, QAB[1][64:128, :], self.qT[h * 128 + 64:(h + 1) * 128, :], writes=[bQ])
                qtiles = tiles_of(0, SL, 256) + [(SL, CTX)]
                for (q0, qs) in qtiles:
                    klist = list(range(NK)) if q0 < SL else list(range(NKL, NK))
                    nqb = qs // 128
                    pend = None

                    def issue_qk(kt, q0=q0, qs=qs):
                        nonlocal git
                        sset = git % 2
                        git += 1
                        def mmqk(sset=sset):
                            ins = None
                            for c in range(2):
                                ins = nc.tensor.matmul(
                                    pS[sset][c][:, 0:qs], KT[hk][:, kt * 128:(kt + 1) * 128],
                                    QAB[c][:, q0:q0 + qs], start=True, stop=True)
                            return ins
                        kb.op(PE, mmqk, reads=[bKT[hk], bQ], writes=[bpS[sset][0]])
                        return sset

                    def issue_exp_pv(kt, sset, first, last, qs=qs, nqb=nqb):
                        nonlocal ecount
                        es_ = ecount % NE
                        ecount += 1
                        for c in range(2):
                            kb.op(ACT, lambda c=c: nc.scalar.activation(out=E[es_][c][:, 0:qs], in_=pS[sset][c][:, 0:qs], func=AF.Exp,
                                                                        scale=QK ** -0.5), reads=[bpS[sset][0]], writes=[bE[es_][c]])
                        for c in range(2):
                            for qb in range(nqb):
                                kb.op(PE, lambda c=c, qb=qb: nc.tensor.matmul(
                                    acc(qb, c)[:, 0:130], E[es_][c][:, qb * 128:(qb + 1) * 128], VX[hk][:, kt, 0:130],
                                    start=first, stop=last), reads=[bE[es_][c], bVX[hk]], writes=[bAcc[qb][c]])

                    prev = None
                    for idx, kt in enumerate(klist):
                        sset = issue_qk(kt)
                        if prev is not None:
                            issue_exp_pv(prev[0], prev[1], prev[2] == 0, False)
                        prev = (kt, sset, idx)
                    issue_exp_pv(prev[0], prev[1], prev[2] == 0, True)
                    ak = ocount % 2
                    for qb in range(nqb):
                        ok = ocount % 2
                        ocount += 1
                        sc = SC[ok]
                        kb.op(DVE, lambda qb=qb, sc=sc: nc.vector.reciprocal(out=sc[:, 0:1], in_=acc(qb, 0)[:, 128:129]), reads=[bAcc[qb][0]], writes=[bSC[ok]])
                        kb.op(DVE, lambda qb=qb, sc=sc: nc.vector.reciprocal(out=sc[:, 1:2], in_=acc(qb, 1)[:, 128:129]), reads=[bAcc[qb][1]], writes=[bSC[ok]])
                        kb.op(DVE, lambda sc=sc: nc.vector.tensor_tensor(out=sc[:, 2:3], in0=sc[:, 1:2], in1=self.LAMT[:, i:i + 1], op=ALU.mult),
                              reads=[bSC[ok]], writes=[bSC[ok]])
                        kb.op(DVE, lambda qb=qb, sc=sc, ok=ok: nc.vector.tensor_scalar(out=O2[ok][:, :], in0=acc(qb, 1)[:, 0:128], scalar1=sc[:, 2:3],
                                                                                      scalar2=None, op0=ALU.mult), reads=[bAcc[qb][1], bSC[ok]], writes=[bO2[ok]])
                        kb.op(DVE, lambda qb=qb, sc=sc, ok=ok: nc.vector.scalar_tensor_tensor(
                            out=OT[ok][:, :], in0=acc(qb, 0)[:, 0:128], scalar=sc[:, 0:1], in1=O2[ok][:, :], op0=ALU.mult, op1=ALU.subtract),
                            reads=[bAcc[qb][0], bSC[ok], bO2[ok]], writes=[bOT[ok]])
                        kb.op(DVE, lambda ok=ok: nc.vector.tensor_tensor(out=SQt[ok][:, :], in0=OT[ok][:, :], in1=OT[ok][:, :], op=ALU.mult),
                              reads=[bOT[ok]], writes=[bSQ[ok]])
                        kb.op(DVE, lambda ok=ok, sc=sc: nc.vector.reduce_sum(out=sc[:, 3:4], in_=SQt[ok][:, :], axis=AX.X), reads=[bSQ[ok]], writes=[bSC[ok]])
                        kb.op(DVE, lambda sc=sc: nc.vector.tensor_scalar(out=sc[:, 4:5], in0=sc[:, 3:4], scalar1=1.0 / (128 * (1 - lam_init) ** 2), scalar2=EPS / (1 - lam_init) ** 2,
                                                                       op0=ALU.mult, op1=ALU.add), reads=[bSC[ok]], writes=[bSC[ok]])
                        kb.op(POOL, lambda sc=sc: nc.gpsimd.tensor_tensor(out=sc[:, 5:6], in0=sc[:, 4:5], in1=negh[:, 0:1], op=ALU.pow),
                              reads=[bSC[ok], bC], writes=[bSC[ok]])
                        kb.op(DVE, lambda ok=ok, sc=sc: nc.vector.scalar_tensor_tensor(
                            out=AB[ok][:, :], in0=OT[ok][:, :], scalar=sc[:, 5:6], in1=SUBG[:, :], op0=ALU.mult, op1=ALU.mult),
                            reads=[bOT[ok], bSC[ok], bC], writes=[bAB[ok]])
                        kb.op(PE, lambda ok=ok, qb=qb: nc.tensor.transpose(pT[:, qb, :], AB[ok][:, :], id_bf), reads=[bAB[ok]], writes=[bpT])
                    kb.op(ACT, lambda ak=ak, qs=qs, nqb=nqb: nc.scalar.activation(
                        out=ATO[ak][:, 0:qs].rearrange("p (b q) -> p b q", q=128), in_=pT[:, 0:nqb, :], func=AF.Identity), reads=[bpT], writes=[bATO[ak]])
                    kb.dma(POOL, self.attT[rows, q0:q0 + qs], ATO[ak][:, 0:qs], reads=[bATO[ak]])
            kb.barrier(self.bar_scr[:, 0:1])

    def phase_sgu(self, i):
        cfg, nc, kb = self.cfg, self.nc, self.kb
        NT = cfg.nt
        PE, ACT, DVE, POOL, SP = kb.PE, kb.ACT, kb.DVE, kb.POOL, kb.SP
        with contextlib.ExitStack() as es:
            WS32 = self.sb(es, "sWS32", [128, 8, 128], F32)
            WS = self.sb(es, "sWS", [128, 8, 128], BF16)
            BS = self.sb(es, "sBS", [128, 8, 128], F32)
            bW = Buf()
            kb.dma(SP, WS32[:], self.wsp_in[i, :, :, :], writes=[bW])
            o, n = SM["bsp"]
            kb.dma(SP, BS[:], self.sm_in[i, :, o:o + n].rearrange("p (g q) -> p g q", g=8), writes=[bW])
            bWS = Buf()
            kb.op(DVE, lambda: nc.vector.tensor_copy(WS[:], WS32[:]), reads=[bW], writes=[bWS])
            VV = [self.sb(es, f"sVV{k}", [128, 1024], BF16) for k in range(2)]
            UT = [self.sb(es, f"sUT{k}", [128, 8, 128], BF16) for k in range(2)]
            M = [self.sb(es, f"sM{k}", [128, 8, 128], F32) for k in range(2)]
            SO = [self.sb(es, f"sSO{k}", [128, 8, 128], BF16) for k in range(2)]
            bVV, bUT, bM, bSO = ([Buf(), Buf()] for _ in range(4))
            pss = [self.ps(es, f"sps{k}", [128, 4, 128], F32) for k in range(4)]
            bps = [Buf() for _ in range(4)]
            pc = 0
            for it, (t0, ts) in enumerate(tiles_of(0, NT, 128)):
                k = it % 2
                kb.dma(SP, VV[k][:], self.vvn[t0:t0 + 128, :], writes=[bVV[k]])
                kb.dma(SP, UT[k][:], self.uT[:, t0:t0 + 128].rearrange("(g c) t -> c g t", c=128), writes=[bUT[k]])
                for half in range(2):
                    pk = pc % 4
                    pc += 1

                    def mm(pk=pk, half=half, k=k):
                        ins = None
                        for gg in range(4):
                            g = half * 4 + gg
                            ins = nc.tensor.matmul(pss[pk][:, gg, :], VV[k][:, g * 128:(g + 1) * 128], WS[:, g, :], start=True, stop=True)
                        return ins
                    kb.op(PE, mm, reads=[bVV[k], bWS], writes=[bps[pk]])
                    kb.op(DVE, lambda pk=pk, half=half, k=k: nc.vector.tensor_tensor(
                        out=M[k][:, half * 4:half * 4 + 4, :], in0=pss[pk][:, :, :], in1=BS[:, half * 4:half * 4 + 4, :], op=ALU.add),
                        reads=[bps[pk], bW], writes=[bM[k]])
                kb.op(POOL, lambda k=k: nc.gpsimd.tensor_tensor(out=SO[k][:], in0=M[k][:], in1=UT[k][:], op=ALU.mult),
                      reads=[bM[k], bUT[k]], writes=[bSO[k]])
                kb.dma(POOL, self.sguT[:, t0:t0 + 128].rearrange("(g c) t -> c g t", c=128), SO[k][:], reads=[bSO[k]])
            kb.barrier(self.bar_scr[:, 0:1])

    def phase_conv(self, i):
        cfg, nc, kb = self.cfg, self.nc, self.kb
        NT, SL = cfg.nt, cfg.s_loc
        PE, ACT, DVE, POOL, SP = kb.PE, kb.ACT, kb.DVE, kb.POOL, kb.SP
        edge_all = self.edge_all_t.ap()
        ones_bf = self.cbf[:, 128:256]
        with contextlib.ExitStack() as es:
            CW = 8
            TS = 512
            HPt = [self.sb(es, f"cHP{k}", [128, CW, TS + 2 * HALO], BF16) for k in range(2)]
            bHPt = [Buf(), Buf()]
            ED = self.sb(es, "cED", [128, GROUP, CW, 32], BF16)
            EF = self.sb(es, "cEF", [128, GROUP, CW, 32], F32)
            HL = self.sb(es, "cHL", [128, CW, 2 * HALO], F32)
            bED = Buf()
            kb.dma(SP, ED[:], edge_all.rearrange("(r j c) e -> c r j e", r=GROUP, c=128), writes=[bED])
            kb.op(DVE, lambda: nc.vector.tensor_copy(EF[:], ED[:]), reads=[bED], writes=[bED])
            for side in range(2):
                src_lo = 17 if side == 0 else 0
                dst = HL[:, :, side * HALO:(side + 1) * HALO]
                for r in range(GROUP):
                    sel = self.consts[:, C_SEL + side * 4 + r:C_SEL + side * 4 + r + 1]
                    if r == 0:
                        kb.op(DVE, lambda dst=dst, r=r, sel=sel, src_lo=src_lo: nc.vector.tensor_scalar(
                            out=dst, in0=EF[:, r, :, src_lo:src_lo + HALO], scalar1=sel, scalar2=None, op0=ALU.mult), reads=[bED], writes=[bED])
                    else:
                        kb.op(DVE, lambda dst=dst, r=r, sel=sel, src_lo=src_lo: nc.vector.scalar_tensor_tensor(
                            out=dst, in0=EF[:, r, :, src_lo:src_lo + HALO], scalar=sel, in1=dst, op0=ALU.mult, op1=ALU.add),
                            reads=[bED], writes=[bED])
            HLb = self.sb(es, "cHLb", [128, CW, 2 * HALO], BF16)
            kb.op(DVE, lambda: nc.vector.tensor_copy(HLb[:], HL[:]), reads=[bED], writes=[bED])
            PRM = self.sb(es, "cPRM", [128, 24 + 8 * CONV_K], F32)
            bP = Buf()
            o, n = SM["convb"]
            kb.dma(SP, PRM[:], self.sm_in[i, :, o:o + 24 + 8 * CONV_K], writes=[bP])
            DG = self.sb(es, "cDG", [128, CW, CONV_K, 128], BF16)
            bDG = Buf()
            for j in range(CW):
                for kk in range(CONV_K):
                    eng = DVE if (kk % 2 == 0) else POOL
                    sc = PRM[:, 24 + j * CONV_K + kk:24 + j * CONV_K + kk + 1]
                    if eng is DVE:
                        kb.op(DVE, lambda j=j, kk=kk, sc=sc: nc.vector.tensor_scalar(
                            out=DG[:, j, kk, :], in0=self.consts[:, C_ID:C_ID + 128], scalar1=sc, scalar2=None, op0=ALU.mult),
                            reads=[bP], writes=[bDG])
                    else:
                        kb.op(POOL, lambda j=j, kk=kk, sc=sc: nc.gpsimd.tensor_scalar(
                            out=DG[:, j, kk, :], in0=self.consts[:, C_ID:C_ID + 128], scalar1=sc, scalar2=None, op0=ALU.mult),
                            reads=[bP], writes=[bDG])
            pss = [self.ps(es, f"cps{k}", [128, TS], F32) for k in range(4)]
            bps = [Buf() for _ in range(4)]
            pst = [self.ps(es, f"cpst{k}", [128, TS], F32) for k in range(2)]
            bpst = [Buf(), Buf()]
            CO = [self.sb(es, f"cCO{k}", [128, CW, TS], F32) for k in range(2)]
            CB = [self.sb(es, "cCB0", [128, CW, TS], BF16)] * 2
            CS = [self.sb(es, "cCS0", [128, CW, TS], BF16)] * 2
            OUT = [self.sb(es, f"cOUT{k}", [128, CW, TS], BF16) for k in range(2)]
            ST = [self.sb(es, f"cST{k}", [128, 3, TS], F32) for k in range(2)]
            T1 = [self.sb(es, f"cT1{k}", [128, TS], F32) for k in range(2)]
            bCO, bOUT, bSTb, bT1 = ([Buf(), Buf()] for _ in range(4))
            _b1, _b2 = Buf(), Buf()
            bCB, bCS = [_b1, _b1], [_b2, _b2]
            negh = self.sb(es, "cnegh", [128, TS], F32)
            kb.op(POOL, lambda: nc.gpsimd.memset(negh[:], -0.5), writes=[bP])
            pc = 0
            t1c = 0
            for it, (t0, ts) in enumerate(tiles_of(0, SL, TS) + [(SL, CTX)]):
                k = it % 2
                src = HPt[k]
                lo = 0
                if t0 < SL:
                    a0 = max(t0 - HALO, 0)
                    a1 = min(t0 + ts + HALO, SL)
                    kb.dma(SP, src[:, :, HALO - (t0 - a0):HALO + (a1 - t0)], self.hcT[:, a0:a1].rearrange("(j c) t -> c j t", c=128), writes=[bHPt[k]])
                    if t0 == 0:
                        kb.op(DVE, lambda src=src: nc.vector.tensor_copy(src[:, :, 0:HALO], HLb[:, :, 0:HALO]), reads=[bED], writes=[bHPt[k]])
                    if t0 + ts == SL:
                        kb.op(DVE, lambda src=src, ts=ts: nc.vector.tensor_copy(src[:, :, HALO + ts:2 * HALO + ts], HLb[:, :, HALO:2 * HALO]),
                              reads=[bED], writes=[bHPt[k]])
                else:
                    kb.op(POOL, lambda src=src: nc.gpsimd.memset(src[:, :, 0:HALO], 0.0), writes=[bHPt[k]])
                    kb.op(POOL, lambda src=src, ts=ts: nc.gpsimd.memset(src[:, :, HALO + ts:2 * HALO + ts], 0.0), writes=[bHPt[k]])
                    kb.dma(SP, src[:, :, HALO:HALO + ts], self.hcT[:, t0:t0 + ts].rearrange("(j c) t -> c j t", c=128), writes=[bHPt[k]])
                for j in range(CW):
                    pk = pc % 4
                    pc += 1

                    def mm(pk=pk, j=j, src=src, lo=lo, ts=ts):
                        ins = None
                        for kk in range(CONV_K):
                            ins = nc.tensor.matmul(pss[pk][:, 0:ts], DG[:, j, kk, :], src[:, j, lo + kk:lo + kk + ts],
                                                   start=(kk == 0), stop=(kk == CONV_K - 1))
                        return ins
                    kb.op(PE, mm, reads=[bDG, bHPt[k]], writes=[bps[pk]])
                    kb.op(ACT, lambda pk=pk, j=j, k=k, ts=ts: nc.scalar.activation(
                        out=CO[k][:, j, 0:ts], in_=pss[pk][:, 0:ts], func=AF.Identity, bias=PRM[:, j:j + 1], scale=1.0),
                        reads=[bps[pk], bP], writes=[bCO[k]])
                    kb.op(POOL, lambda j=j, k=k, ts=ts: nc.gpsimd.tensor_copy(CB[k][:, j, 0:ts], CO[k][:, j, 0:ts]), reads=[bCO[k]], writes=[bCB[k]])
                    kb.op(DVE, lambda j=j, k=k, ts=ts: nc.vector.tensor_tensor(out=CS[k][:, j, 0:ts], in0=CO[k][:, j, 0:ts], in1=CO[k][:, j, 0:ts],
                                                                              op=ALU.mult), reads=[bCO[k]], writes=[bCS[k]])
                for s_i, (srcb, bsrc) in enumerate(((CB, bCB), (CS, bCS))):
                    def mm(srcb=srcb, s_i=s_i, k=k, ts=ts):
                        ins = None
                        for j in range(CW):
                            ins = nc.tensor.matmul(pst[s_i][:, 0:ts], ones_bf, srcb[k][:, j, 0:ts], start=(j == 0), stop=(j == CW - 1))
                        return ins
                    kb.op(PE, mm, reads=[bsrc[k]], writes=[bpst[s_i]])
                st = ST[k]
                kb.op(DVE, lambda st=st, ts=ts: nc.vector.tensor_scalar(out=st[:, 0, 0:ts], in0=pst[0][:, 0:ts], scalar1=1.0 / CONV_W, scalar2=None,
                                                                      op0=ALU.mult), reads=[bpst[0]], writes=[bSTb[k]])
                kb.op(DVE, lambda st=st, ts=ts: nc.vector.tensor_tensor(out=st[:, 1, 0:ts], in0=st[:, 0, 0:ts], in1=st[:, 0, 0:ts], op=ALU.mult),
                      reads=[bSTb[k]], writes=[bSTb[k]])
                kb.op(DVE, lambda st=st, ts=ts: nc.vector.scalar_tensor_tensor(out=st[:, 2, 0:ts], in0=pst[1][:, 0:ts], scalar=1.0 / CONV_W,
                                                                             in1=st[:, 1, 0:ts], op0=ALU.mult, op1=ALU.subtract),
                      reads=[bpst[1], bSTb[k]], writes=[bSTb[k]])
                kb.op(DVE, lambda st=st, ts=ts: nc.vector.tensor_scalar(out=st[:, 1, 0:ts], in0=st[:, 2, 0:ts], scalar1=EPS, scalar2=None, op0=ALU.add),
                      reads=[bSTb[k]], writes=[bSTb[k]])
                kb.op(POOL, lambda st=st, ts=ts: nc.gpsimd.tensor_tensor(out=st[:, 2, 0:ts], in0=st[:, 1, 0:ts], in1=negh[:, 0:ts], op=ALU.pow),
                      reads=[bSTb[k], bP], writes=[bSTb[k]])
                for j in range(CW):
                    tk = t1c % 2
                    t1c += 1
                    kb.op(DVE, lambda j=j, k=k, tk=tk, st=st, ts=ts: nc.vector.tensor_tensor(
                        out=T1[tk][:, 0:ts], in0=CO[k][:, j, 0:ts], in1=st[:, 0, 0:ts], op=ALU.subtract), reads=[bCO[k], bSTb[k]], writes=[bT1[tk]])
                    kb.op(DVE, lambda tk=tk, st=st, ts=ts: nc.vector.tensor_tensor(
                        out=T1[tk][:, 0:ts], in0=T1[tk][:, 0:ts], in1=st[:, 2, 0:ts], op=ALU.mult), reads=[bT1[tk], bSTb[k]], writes=[bT1[tk]])
                    kb.op(ACT, lambda j=j, k=k, tk=tk, ts=ts: nc.scalar.activation(
                        out=OUT[k][:, j, 0:ts], in_=T1[tk][:, 0:ts], func=AF.Silu, bias=PRM[:, 16 + j:17 + j], scale=PRM[:, 8 + j:9 + j]),
                        reads=[bT1[tk], bP], writes=[bOUT[k]])
                kb.dma(POOL, self.convT[:, t0:t0 + ts].rearrange("(j c) t -> c j t", c=128), OUT[k][:, :, 0:ts], reads=[bOUT[k]])
            kb.barrier(self.bar_scr[:, 0:1])

    def phase_merge(self, i):
        cfg, nc, kb = self.cfg, self.nc, self.kb
        NT, SL = cfg.nt, cfg.s_loc
        PE, ACT, DVE, POOL, SP = kb.PE, kb.ACT, kb.DVE, kb.POOL, kb.SP
        WG, WO = ("ga", i), ("o", i)
        WBR = [("ao", i), ("so", i), ("co", i)]
        BRS = [self.attT, self.sguT, self.convT]
        STS = 512
        with contextlib.ExitStack() as es:
            HT = self.sb(es, "mHT", [128, DC, STS], BF16)
            BR = [self.sb(es, f"mBR{b}", [128, 8, STS], BF16) for b in range(3)]
            YT = self.sb(es, "mYT", [128, DC, STS], BF16)
            bHT, bYT = Buf(), Buf()
            bBR = [Buf() for _ in range(3)]
            WGt = [self.sb(es, f"mWG{k}", [128, 3, DC, 128], BF16) for k in range(2)]
            WBt = [self.sb(es, f"mWB{k}", [128, 3, 8, 128], BF16) for k in range(2)]
            WOt = [self.sb(es, f"mWO{k}", [128, DC, 128], BF16) for k in range(2)]
            bWG, bWB, bWO = ([Buf(), Buf()] for _ in range(3))
            BG = self.sb(es, "mBG", [128, 48], F32)
            bBG = Buf()
            o, n = SM["bgate"]
            kb.dma(SP, BG[:], self.sm_in[i, :, o:o + n], writes=[bBG])
            pss = [self.ps(es, f"mps{k}", [128, 512], F32) for k in range(8)]
            bps = [Buf() for _ in range(8)]
            SG = [self.sb(es, f"mSG{k}", [128, 3, 512], F32) for k in range(2)]
            PR = [self.sb(es, f"mPR{k}", [128, 3, 512], F32) for k in range(2)]
            bSG, bPR = [Buf(), Buf()], [Buf(), Buf()]
            XL = [self.sb(es, f"mXL{k}", [128, 512], F32) for k in range(3)]
            bXL = [Buf() for _ in range(3)]
            pc = 0
            ec = 0
            xc = 0
            wc = 0
            for (s0, ssz) in tiles_of(0, NT, STS):
                kb.dma(SP, HT[:, :, 0:ssz], self.hT[:, s0:s0 + ssz].rearrange("(c p) t -> p c t", p=128), writes=[bHT])
                for b in range(3):
                    kb.dma(SP, BR[b][:, :, 0:ssz], BRS[b][:, s0:s0 + ssz].rearrange("(c p) t -> p c t", p=128), writes=[bBR[b]])
                ttiles = tiles_of(s0, s0 + ssz, 512)
                for m in range(DC):
                    wk = wc % 2
                    wc += 1
                    SP.wait(self.cast_ev[i])
                    for b in range(3):
                        self.wload(WGt[wk][:, b, :, :], WG, 0, DC, b * D + m * 128, 128, bWG[wk], i)
                        self.wload(WBt[wk][:, b, :, :], WBR[b], 0, 8, m * 128, 128, bWB[wk], i)
                    for (t0, ts) in ttiles:
                        lo = t0 - s0
                        pg, pb = [], []
                        for b in range(3):
                            pk = pc % 8
                            pc += 1
                            pg.append(pk)

                            def mm(pk=pk, b=b, wk=wk, lo=lo, ts=ts):
                                ins = None
                                for c in range(DC):
                                    ins = nc.tensor.matmul(pss[pk][:, 0:ts], WGt[wk][:, b, c, :], HT[:, c, lo:lo + ts], start=(c == 0), stop=(c == DC - 1))
                                return ins
                            kb.op(PE, mm, reads=[bWG[wk], bHT], writes=[bps[pk]])
                        for b in range(3):
                            pk = pc % 8
                            pc += 1
                            pb.append(pk)

                            def mm(pk=pk, b=b, wk=wk, lo=lo, ts=ts):
                                ins = None
                                for c in range(8):
                                    ins = nc.tensor.matmul(pss[pk][:, 0:ts], WBt[wk][:, b, c, :], BR[b][:, c, lo:lo + ts], start=(c == 0), stop=(c == 7))
                                return ins
                            kb.op(PE, mm, reads=[bWB[wk], bBR[b]], writes=[bps[pk]])
                        ek = ec % 2
                        ec += 1
                        for b in range(3):
                            kb.op(ACT, lambda b=b, ek=ek, ts=ts, pk=pg[b], m=m: nc.scalar.activation(
                                out=SG[ek][:, b, 0:ts], in_=pss[pk][:, 0:ts], func=AF.Sigmoid, bias=BG[:, b * 16 + m:b * 16 + m + 1], scale=1.0),
                                reads=[bps[pg[b]], bBG], writes=[bSG[ek]])
                        for b in range(3):
                            kb.op(DVE, lambda b=b, ek=ek, ts=ts, pk=pb[b]: nc.vector.tensor_tensor(
                                out=PR[ek][:, b, 0:ts], in0=pss[pk][:, 0:ts], in1=SG[ek][:, b, 0:ts], op=ALU.mult),
                                reads=[bps[pb[b]], bSG[ek]], writes=[bPR[ek]])
                        kb.op(POOL, lambda ek=ek, ts=ts: nc.gpsimd.tensor_tensor(out=PR[ek][:, 0, 0:ts], in0=PR[ek][:, 0, 0:ts], in1=PR[ek][:, 1, 0:ts],
                                                                                 op=ALU.add), reads=[bPR[ek]], writes=[bPR[ek]])
                        kb.op(POOL, lambda ek=ek, ts=ts, m=m, lo=lo: nc.gpsimd.tensor_tensor(
                            out=YT[:, m, lo:lo + ts], in0=PR[ek][:, 0, 0:ts], in1=PR[ek][:, 2, 0:ts], op=ALU.add), reads=[bPR[ek]], writes=[bYT])
                col_of = lambda t0: 0 if t0 < SL else 1
                for m in range(DC):
                    wk = wc % 2
                    wc += 1
                    self.wload(WOt[wk][:], WO, 0, DC, m * 128, 128, bWO[wk], i)
                    for (t0, ts) in ttiles:
                        for (u0, us) in ([(t0, ts)] if not (t0 < SL < t0 + ts) else [(t0, SL - t0), (SL, t0 + ts - SL)]):
                            lo = u0 - s0
                            pk = pc % 8
                            pc += 1

                            def mm(pk=pk, wk=wk, lo=lo, us=us):
                                ins = None
                                for c in range(DC):
                                    ins = nc.tensor.matmul(pss[pk][:, 0:us], WOt[wk][:, c, :], YT[:, c, lo:lo + us], start=(c == 0), stop=(c == DC - 1))
                                return ins
                            kb.op(PE, mm, reads=[bWO[wk], bYT], writes=[bps[pk]])
                            xk = xc % 3
                            xc += 1
                            kb.dma(SP, XL[xk][:, 0:us], self.xT[m * 128:(m + 1) * 128, u0:u0 + us], writes=[bXL[xk]])
                            col = col_of(u0)
                            kb.op(DVE, lambda pk=pk, xk=xk, us=us, m=m, col=col: nc.vector.scalar_tensor_tensor(
                                out=XL[xk][:, 0:us], in0=pss[pk][:, 0:us], scalar=self.MOD[:, i, 32 + m, col:col + 1], in1=XL[xk][:, 0:us],
                                op0=ALU.mult, op1=ALU.add), reads=[bps[pk], bXL[xk]], writes=[bXL[xk]])
                            kb.dma(POOL, self.xT[m * 128:(m + 1) * 128, u0:u0 + us], XL[xk][:, 0:us], reads=[bXL[xk]])
            kb.barrier(self.bar_scr[:, 0:1])

    def phase_ffn(self, i):
        cfg, nc, kb = self.cfg, self.nc, self.kb
        NT, SL = cfg.nt, cfg.s_loc
        PE, ACT, DVE, POOL, SP = kb.PE, kb.ACT, kb.DVE, kb.POOL, kb.SP
        moe = (i % 2 == 1)
        if moe:
            experts = [(("m1", i, e), ("m3", i, e), ("m2", i, e), 0, 0) for e in range(NEXP)]
        else:
            experts = [(("f1", i), ("f3", i), ("f2", i), hh * EDIM, hh * EDIM) for hh in range(2)]
        STS = 1024
        ones32 = self.consts[:, C_ONES:C_ONES + 128]
        with contextlib.ExitStack() as es:
            HT = self.sb(es, "fHT", [128, DC, STS], BF16)
            HID = self.sb(es, "fHID", [128, EC, STS], BF16)
            bHT, bHID = Buf(), Buf()
            W1 = [self.sb(es, f"fW1{k}", [128, DC, 256], BF16) for k in range(2)]
            W3 = [self.sb(es, f"fW3{k}", [128, DC, 256], BF16) for k in range(2)]
            W2 = [self.sb(es, f"fW2{k}", [128, EC, 256], BF16) for k in range(2)]
            bW1, bW3, bW2 = ([Buf(), Buf()] for _ in range(3))
            pss = [self.ps(es, f"fps{k}", [128, 512], F32) for k in range(7)]
            bps = [Buf() for _ in range(7)]
            psg = self.ps(es, "fpsg", [128, 512], F32)
            bpsg = Buf()
            SS = [self.sb(es, f"fSS{k}", [128, 512], F32) for k in range(3)]
            HH = [self.sb(es, f"fHH{k}", [128, 512], F32) for k in range(3)]
            bSS, bHH = [Buf() for _ in range(3)], [Buf() for _ in range(3)]
            XL = [self.sb(es, f"fXL{k}", [128, 512], F32) for k in range(3)]
            bXL = [Buf() for _ in range(3)]
            if moe:
                GT = self.sb(es, "fGT", [128, STS // 128, NEXP], F32)
                GE = self.sb(es, "fGE", [128, STS], F32)
                DGm = [self.sb(es, f"fDG{k}", [128, 128], F32) for k in range(2)]
                bGT, bGE = Buf(), Buf()
                bDGm = [Buf(), Buf()]
            pc = sc = xc = wc1 = wc2 = dc = 0
            for (s0, ssz) in tiles_of(0, NT, STS):
                kb.dma(SP, HT[:, :, 0:ssz], self.hT[:, s0:s0 + ssz].rearrange("(c p) t -> p c t", p=128), writes=[bHT])
                ttiles = tiles_of(s0, s0 + ssz, 512)
                xbuf = {}
                if moe:
                    kb.dma(SP, GT[:, 0:ssz // 128, :], self.gate_d[s0:s0 + ssz, :].rearrange("(b p) e -> p b e", p=128), writes=[bGT])
                for e, (w1, w3, w2, c0, r0) in enumerate(experts):
                    if moe:
                        for b in range(ssz // 128):
                            dk = dc % 2
                            dc += 1
                            kb.op(DVE, lambda dk=dk, b=b, e=e: nc.vector.tensor_scalar(
                                out=DGm[dk][:, :], in0=self.consts[:, C_ID:C_ID + 128], scalar1=GT[:, b, e:e + 1], scalar2=None, op0=ALU.mult),
                                reads=[bGT], writes=[bDGm[dk]])
                            kb.op(PE, lambda dk=dk, b=b: nc.tensor.matmul(psg[:, (b % 4) * 128:(b % 4 + 1) * 128], ones32, DGm[dk][:, :], start=True, stop=True),
                                  reads=[bDGm[dk]], writes=[bpsg])
                            if b % 4 == 3 or b == ssz // 128 - 1:
                                b0 = (b // 4) * 4
                                nb = b - b0 + 1
                                kb.op(ACT, lambda b0=b0, nb=nb: nc.scalar.activation(out=GE[:, b0 * 128:(b0 + nb) * 128], in_=psg[:, 0:nb * 128], func=AF.Identity),
                                      reads=[bpsg], writes=[bGE])
                    for n0 in range(0, EDIM, 256):
                        wk = wc1 % 2
                        wc1 += 1
                        self.wload(W1[wk][:], w1, 0, DC, c0 + n0, 256, bW1[wk], i)
                        self.wload(W3[wk][:], w3, 0, DC, c0 + n0, 256, bW3[wk], i)
                        for wcn in range(2):
                            j = n0 // 128 + wcn
                            for (t0, ts) in ttiles:
                                lo = t0 - s0
                                pks = []
                                for (Wt, bWt) in ((W1, bW1), (W3, bW3)):
                                    pk = pc % 7
                                    pc += 1
                                    pks.append(pk)

                                    def mm(pk=pk, Wt=Wt, wk=wk, wcn=wcn, lo=lo, ts=ts):
                                        ins = None
                                        for c in range(DC):
                                            ins = nc.tensor.matmul(pss[pk][:, 0:ts], Wt[wk][:, c, wcn * 128:(wcn + 1) * 128], HT[:, c, lo:lo + ts],
                                                                   start=(c == 0), stop=(c == DC - 1))
                                        return ins
                                    kb.op(PE, mm, reads=[bWt[wk], bHT], writes=[bps[pk]])
                                sk = sc % 3
                                sc += 1
                                kb.op(ACT, lambda sk=sk, pk=pks[0], ts=ts: nc.scalar.activation(out=SS[sk][:, 0:ts], in_=pss[pk][:, 0:ts], func=AF.Silu),
                                      reads=[bps[pks[0]]], writes=[bSS[sk]])
                                if moe:
                                    kb.op(DVE, lambda sk=sk, pk=pks[1], ts=ts: nc.vector.tensor_tensor(
                                        out=HH[sk][:, 0:ts], in0=pss[pk][:, 0:ts], in1=SS[sk][:, 0:ts], op=ALU.mult),
                                        reads=[bps[pks[1]], bSS[sk]], writes=[bHH[sk]])
                                    kb.op(POOL, lambda sk=sk, j=j, lo=lo, ts=ts: nc.gpsimd.tensor_tensor(
                                        out=HID[:, j, lo:lo + ts], in0=HH[sk][:, 0:ts], in1=GE[:, lo:lo + ts], op=ALU.mult),
                                        reads=[bHH[sk], bGE], writes=[bHID])
                                else:
                                    kb.op(DVE, lambda sk=sk, pk=pks[1], j=j, lo=lo, ts=ts: nc.vector.tensor_tensor(
                                        out=HID[:, j, lo:lo + ts], in0=pss[pk][:, 0:ts], in1=SS[sk][:, 0:ts], op=ALU.mult),
                                        reads=[bps[pks[1]], bSS[sk]], writes=[bHID])
                    for n0 in range(0, D, 256):
                        wk = wc2 % 2
                        wc2 += 1
                        self.wload(W2[wk][:], w2, r0, EC, n0, 256, bW2[wk], i)
                        for wcn in range(2):
                            m = n0 // 128 + wcn
                            for (t0, ts) in ttiles:
                                for (u0, us) in ([(t0, ts)] if not (t0 < SL < t0 + ts) else [(t0, SL - t0), (SL, t0 + ts - SL)]):
                                    lo = u0 - s0
                                    pk = pc % 7
                                    pc += 1

                                    def mm(pk=pk, wk=wk, wcn=wcn, lo=lo, us=us):
                                        ins = None
                                        for c in range(EC):
                                            ins = nc.tensor.matmul(pss[pk][:, 0:us], W2[wk][:, c, wcn * 128:(wcn + 1) * 128], HID[:, c, lo:lo + us],
                                                                   start=(c == 0), stop=(c == EC - 1))
                                        return ins
                                    kb.op(PE, mm, reads=[bW2[wk], bHID], writes=[bps[pk]])
                                    xk = xc % 3
                                    xc += 1
                                    dkey = (m, u0)
                                    if dkey not in xbuf:
                                        xbuf[dkey] = Buf()
                                    bd = xbuf[dkey]
                                    kb.dma(SP, XL[xk][:, 0:us], self.xT[m * 128:(m + 1) * 128, u0:u0 + us], reads=[bd], writes=[bXL[xk]], sb=bXL[xk])
                                    col = 0 if u0 < SL else 1
                                    kb.op(DVE, lambda pk=pk, xk=xk, us=us, m=m, col=col: nc.vector.scalar_tensor_tensor(
                                        out=XL[xk][:, 0:us], in0=pss[pk][:, 0:us], scalar=self.MOD[:, i, 80 + m, col:col + 1], in1=XL[xk][:, 0:us],
                                        op0=ALU.mult, op1=ALU.add), reads=[bps[pk], bXL[xk]], writes=[bXL[xk]])
                                    kb.dma(POOL, self.xT[m * 128:(m + 1) * 128, u0:u0 + us], XL[xk][:, 0:us], reads=[bXL[xk]], writes=[bd], sb=bXL[xk])
            kb.barrier(self.bar_scr[:, 0:1])

    def phase_final(self):
        cfg, nc, kb = self.cfg, self.nc, self.kb
        SL = cfg.s_loc
        PE, ACT, DVE, POOL, SP = kb.PE, kb.ACT, kb.DVE, kb.POOL, kb.SP
        ones_bf = self.cbf[:, 128:256]
        with contextlib.ExitStack() as es:
            TS = 256
            X = [self.sb(es, f"zX{k}", [128, DC, TS], F32) for k in range(2)]
            SQ = [self.sb(es, f"zSQ{k}", [128, DC, TS], BF16) for k in range(2)]
            Y = [self.sb(es, f"zY{k}", [128, DC, TS], F32) for k in range(2)]
            RS = [self.sb(es, f"zRS{k}", [128, TS], F32) for k in range(2)]
            negh = self.sb(es, "znegh", [128, TS], F32)
            pss = [self.ps(es, f"zps{k}", [128, TS], F32) for k in range(2)]
            bX, bSQ, bY, bRS, bps = ([Buf(), Buf()] for _ in range(5))
            bneg = Buf()
            kb.op(POOL, lambda: nc.gpsimd.memset(negh[:], -0.5), writes=[bneg])
            fin_sem = None
            for it, (t0, ts) in enumerate(tiles_of(0, SL, TS)):
                k = it % 2
                kb.dma(SP, X[k][:, :, 0:ts], self.xT[:, t0:t0 + ts].rearrange("(c p) t -> p c t", p=128), writes=[bX[k]])
                for c4 in range(0, DC, 4):
                    kb.op(ACT, lambda k=k, c4=c4, ts=ts: nc.scalar.activation(
                        out=SQ[k][:, c4:c4 + 4, 0:ts], in_=X[k][:, c4:c4 + 4, 0:ts], func=AF.Square), reads=[bX[k]], writes=[bSQ[k]])

                def mm(k=k, ts=ts):
                    ins = None
                    for c in range(DC):
                        ins = nc.tensor.matmul(pss[k][:, 0:ts], ones_bf, SQ[k][:, c, 0:ts], start=(c == 0), stop=(c == DC - 1))
                    return ins
                kb.op(PE, mm, reads=[bSQ[k]], writes=[bps[k]])
                kb.op(DVE, lambda k=k, ts=ts: nc.vector.tensor_scalar(
                    out=RS[k][:, 0:ts], in0=pss[k][:, 0:ts], scalar1=1.0 / D, scalar2=EPS, op0=ALU.mult, op1=ALU.add),
                    reads=[bps[k]], writes=[bRS[k]])
                kb.op(POOL, lambda k=k, ts=ts: nc.gpsimd.tensor_tensor(
                    out=RS[k][:, 0:ts], in0=RS[k][:, 0:ts], in1=negh[:, 0:ts], op=ALU.pow), reads=[bRS[k], bneg], writes=[bRS[k]])
                for c in range(DC):
                    kb.op(DVE, lambda k=k, c=c, ts=ts: nc.vector.scalar_tensor_tensor(
                        out=Y[k][:, c, 0:ts], in0=X[k][:, c, 0:ts], scalar=self.consts[:, C_FING + c:C_FING + c + 1],
                        in1=RS[k][:, 0:ts], op0=ALU.mult, op1=ALU.mult), reads=[bX[k], bRS[k]], writes=[bY[k]])
                kb.dma(POOL, self.out[:, t0:t0 + ts].rearrange("(c p) t -> p c t", p=128), Y[k][:, :, 0:ts], reads=[bY[k]])
            kb.barrier(self.bar_scr[:, 0:1])


def _feat(v):
    v = np.asarray(v, np.float32)
    return np.ascontiguousarray(v.reshape(-1, 128).T)


def _rope_tables(cfg, r):
    SL, NT = cfg.s_loc, cfg.nt
    n = np.arange(r * SL, (r + 1) * SL, dtype=np.int64)
    row = (n // GRID_W).astype(np.float32)
    col = (n % GRID_W).astype(np.float32)
    half = QK // 2
    inv = (np.float32(ROPE_BASE) ** (-np.arange(0, half, 2, dtype=np.float32) / np.float32(half))).astype(np.float32)
    ang_r = row[:, None] * inv[None, :]
    ang_c = col[:, None] * inv[None, :]
    cosT = np.ones((128, NT), np.float32)
    sinT = np.zeros((128, NT), np.float32)
    for p in range(128):
        hf = (p % 64) // 32
        pair = (p % 32) // 16
        j = p % 16
        ang = ang_r[:, j] if hf == 0 else ang_c[:, j]
        cosT[p, :SL] = np.cos(ang)
        sinT[p, :SL] = (-np.sin(ang)) if pair == 0 else np.sin(ang)
    return cosT, sinT


def make_in_maps(cfg, inp):
    DEPTH, SL, NT = cfg.depth, cfg.s_loc, cfg.nt
    f32 = lambda a: np.ascontiguousarray(np.asarray(a, np.float32))
    x, c, ctx, c_ctx = f32(inp["x"]), f32(inp["c"]), f32(inp["ctx"]), f32(inp["c_ctx"])
    small = np.zeros((DEPTH, 128, SM["_w"]), np.float32)

    def put(i, name, arr):
        o, n = SM[name]
        small[i, :, o:o + n] = arr
    for i in range(DEPTH):
        lam_init = 0.8 - 0.6 * math.exp(-0.3 * i)
        put(i, "bmod", _feat(inp["b_mod"][i]))
        put(i, "g1", _feat(inp["norm1_g"][i]))
        put(i, "g2", _feat(inp["norm2_g"][i]))
        put(i, "bgate", _feat(inp["b_gate"][i]))
        put(i, "convb", _feat(inp["conv_b"][i]))
        put(i, "clng", _feat(inp["conv_ln_g"][i]))
        put(i, "clnb", _feat(inp["conv_ln_b"][i]))
        cw = f32(inp["conv_w"][i])
        put(i, "convw", np.ascontiguousarray(cw.T.reshape(8, 128, CONV_K).transpose(1, 0, 2)).reshape(128, 8 * CONV_K))
        put(i, "subg", np.broadcast_to(f32(inp["subln_g"][i])[None, :], (128, 128)))
        put(i, "slng", np.broadcast_to(f32(inp["sgu_ln_g"][i])[None, :], (128, 1024)))
        put(i, "slnb", np.broadcast_to(f32(inp["sgu_ln_b"][i])[None, :], (128, 1024)))
        put(i, "bsp", np.broadcast_to(f32(inp["b_spatial"][i]).reshape(1, 1024), (128, 1024)))
        lamv = np.concatenate([f32(inp["lam_q1"][i]), f32(inp["lam_k1"][i]), f32(inp["lam_q2"][i]), f32(inp["lam_k2"][i])])
        put(i, "lam", np.broadcast_to(lamv[None, :], (128, 256)))
        if i % 2 == 1:
            put(i, "rb", np.broadcast_to(f32(inp["router_b"][i // 2])[None, :], (128, 8)))
    wspT = np.ascontiguousarray(f32(inp["w_spatial"]).transpose(0, 3, 1, 2))
    shared = {"small": small, "wspT": wspT}
    big = {"w_mod": f32(inp["w_mod"]), "w_in": f32(inp["w_in"]), "w_att_out": f32(inp["w_att_out"]),
           "w_sgu_out": f32(inp["w_sgu_out"]), "w_conv_out": f32(inp["w_conv_out"]), "w_gate": f32(inp["w_gate"]),
           "w_o": f32(inp["w_o"]), "ffn_w1": f32(inp["ffn_w1"]), "ffn_w3": f32(inp["ffn_w3"]), "ffn_w2": f32(inp["ffn_w2"])}
    if cfg.n_moe:
        rw = f32(inp["router_w"])
        shared["router_wT"] = np.ascontiguousarray(rw.reshape(cfg.n_moe, DC, 128, NEXP).transpose(0, 2, 1, 3))
        for k in ("moe_w1", "moe_w3", "moe_w2"):
            big[k] = f32(inp[k])
    rsw = np.zeros((128, 128), np.float32)
    for m in range(128):
        pair = (m % 32) // 16
        k = m + 16 if pair == 0 else m - 16
        rsw[k, m] = 1.0
    in_maps = []
    for core in range(NCORES):
        b, r = core // GROUP, core % GROUP
        xT = np.empty((D, NT), np.float32)
        xT[:, :SL] = x[b, r * SL:(r + 1) * SL, :].T
        xT[:, SL:] = ctx[b].T
        cT = np.stack([_feat(c[b]), _feat(c_ctx)], axis=-1)
        cosT, sinT = _rope_tables(cfg, r)
        consts = np.zeros((128, C_W), np.float32)
        consts[:, C_ID:C_ID + 128] = np.eye(128, dtype=np.float32)
        consts[:, C_ONES:C_ONES + 128] = 1.0
        consts[:, C_RSW:C_RSW + 128] = rsw
        if r > 0:
            consts[:, C_SEL + r - 1] = 1.0
        if r < GROUP - 1:
            consts[:, C_SEL + 4 + r + 1] = 1.0
        consts[:, C_NEGH] = -0.5
        consts[:, C_FING:C_FING + 16] = _feat(inp["final_g"])
        m = dict(shared)
        for k, a in big.items():
            if k == "w_mod":
                cw = a.shape[2] // GROUP
                m[k] = np.ascontiguousarray(a[:, :, r * cw:(r + 1) * cw])
            else:
                rows = a.shape[-2] // GROUP
                m[k] = np.ascontiguousarray(a[..., r * rows:(r + 1) * rows, :])
        m.update({"xT_in": xT, "cT": np.ascontiguousarray(cT), "ropec": cosT, "ropes": sinT, "consts": consts})
        in_maps.append(m)
    return in_maps


_PROG_CACHE = {}


def run(cfg, inp):
    key = (cfg.seq, cfg.depth)
    if key not in _PROG_CACHE:
        _PROG_CACHE[key] = Prog(cfg)
    prog = _PROG_CACHE[key]
    in_maps = make_in_maps(cfg, inp)
    res = run_bass_kernel_spmd(prog.nc, in_maps, core_ids=list(range(NCORES)))
    SL = cfg.s_loc
    out = np.empty((BATCH, cfg.seq, D), np.float32)
    for core in range(NCORES):
        b, r = core // GROUP, core % GROUP
        out[b, r * SL:(r + 1) * SL, :] = np.asarray(res.results[core]["outT"]).T
    return out


def kernel(**inputs):
    return run(Cfg(16384, 4), inputs)
```
